# Optimizing a Trainium2 kernel written in Bass

```python
import jax, jax.numpy as jnp
from jax import lax
import numpy as np

D_MODEL = 1024
BATCH = 32
SEQ = 2048
DEPTH = 1

CTX_LEN = 256
GRID_W = 64
N_HEADS = 8
HEAD_DIM = 64
D_ATT = N_HEADS * HEAD_DIM
WIN_ROWS_MAX = 8
WIN_COLS = 16
D_CONV = 512
CONV_WIDTH = 31
N_GROUPS = 4
EXPERTS_PER_GROUP = 8
N_EXPERTS = N_GROUPS * EXPERTS_PER_GROUP
TOP_K_IN_GROUP = 2
D_EXPERT = 512
MOE_BLOCK = 128
NORM_EPS = 1e-6
NEG_INF = -1e30

Q0 = 0
K0 = Q0 + D_ATT
V0 = K0 + D_ATT
GLU0 = V0 + D_ATT
GA0 = GLU0 + 2 * D_CONV
GB0 = GA0 + D_MODEL
D_IN = GB0 + D_MODEL

kernel_name = "hybrid_natten_conformer_hmoe_dit"


def rmsnorm(x, g):
    xf = x.astype(jnp.float32)
    y = xf * lax.rsqrt(jnp.mean(xf * xf, axis=-1, keepdims=True) + NORM_EPS)
    return (y * g.astype(jnp.float32)).astype(x.dtype)


def layernorm(x, g, b):
    xf = x.astype(jnp.float32)
    mu = jnp.mean(xf, axis=-1, keepdims=True)
    var = jnp.mean(jnp.square(xf - mu), axis=-1, keepdims=True)
    y = (xf - mu) * lax.rsqrt(var + NORM_EPS)
    return (y * g.astype(jnp.float32) + b.astype(jnp.float32)).astype(x.dtype)


def modulate(h, shift, scale):
    return h * (1 + scale) + shift


def split_heads(t):
    return t.reshape(t.shape[0], t.shape[1], N_HEADS, HEAD_DIM)


def latent_neighbourhood_attention(q, k, v, k_ctx, v_ctx, rpb):
    B, N, H, hd = q.shape
    rows = N // GRID_W
    kh = min(WIN_ROWS_MAX, rows)
    scale = hd ** -0.5
    q_rows = q.reshape(B, rows, GRID_W, H, hd).transpose(1, 0, 3, 2, 4)
    k_grid = k.reshape(B, rows, GRID_W, H, hd).transpose(0, 3, 1, 2, 4)
    v_grid = v.reshape(B, rows, GRID_W, H, hd).transpose(0, 3, 1, 2, 4)
    cq = jnp.arange(GRID_W)[:, None]
    ck = jnp.arange(GRID_W)[None, :]
    c_start = jnp.clip(cq - WIN_COLS // 2, 0, GRID_W - WIN_COLS)
    col_mask = (ck >= c_start) & (ck < c_start + WIN_COLS)
    band_mask = jnp.broadcast_to(col_mask[:, None, :], (GRID_W, kh, GRID_W)).reshape(GRID_W, kh * GRID_W)
    dc_idx = jnp.clip(ck - cq + WIN_COLS - 1, 0, 2 * WIN_COLS - 2)
    rpb_cols = rpb[:, :, dc_idx]

    def row_block(args):
        q_r, r = args
        r_start = jnp.clip(r - kh // 2, 0, rows - kh)
        k_blk = lax.dynamic_slice_in_dim(k_grid, r_start, kh, axis=2).reshape(B, H, kh * GRID_W, hd)
        v_blk = lax.dynamic_slice_in_dim(v_grid, r_start, kh, axis=2).reshape(B, H, kh * GRID_W, hd)
        dr_idx = r_start + jnp.arange(kh) - r + WIN_ROWS_MAX - 1
        bias = jnp.take(rpb_cols, dr_idx, axis=1)
        bias = bias.transpose(0, 2, 1, 3).reshape(H, GRID_W, kh * GRID_W).astype(jnp.float32)
        s_loc = jnp.einsum('bhqd,bhkd->bhqk', q_r, k_blk, preferred_element_type=jnp.float32) * scale + bias
        s_loc = jnp.where(band_mask, s_loc, NEG_INF)
        s_ctx = jnp.einsum('bhqd,bhkd->bhqk', q_r, k_ctx, preferred_element_type=jnp.float32) * scale
        p = jax.nn.softmax(jnp.concatenate([s_loc, s_ctx], axis=-1), axis=-1).astype(v.dtype)
        n_loc = kh * GRID_W
        return (jnp.einsum('bhqk,bhkd->bhqd', p[..., :n_loc], v_blk)
                + jnp.einsum('bhqk,bhkd->bhqd', p[..., n_loc:], v_ctx))

    o = lax.map(row_block, (q_rows, jnp.arange(rows)))
    return o.transpose(1, 0, 3, 2, 4).reshape(B, N, H * hd)


def context_attention(q_c, k_c, v_c):
    s = jnp.einsum('bhqd,bhkd->bhqk', q_c, k_c, preferred_element_type=jnp.float32) * (HEAD_DIM ** -0.5)
    p = jax.nn.softmax(s, axis=-1).astype(v_c.dtype)
    o = jnp.einsum('bhqk,bhkd->bhqd', p, v_c)
    return o.transpose(0, 2, 1, 3).reshape(q_c.shape[0], q_c.shape[2], D_ATT)


def conformer_conv(glu_in, conv_w, conv_b, ln_g, ln_b, w_out):
    a, g = jnp.split(glu_in, 2, axis=-1)
    u = a * jax.nn.sigmoid(g)
    y = lax.conv_general_dilated(u, conv_w[:, None, :].astype(u.dtype), window_strides=(1,),
                                 padding=[(CONV_WIDTH // 2, CONV_WIDTH // 2)],
                                 dimension_numbers=('NWC', 'WIO', 'NWC'),
                                 feature_group_count=D_CONV) + conv_b
    y = jax.nn.silu(layernorm(y, ln_g, ln_b))
    return y @ w_out


def merge_branches(proj, att, conv_w, conv_b, ln_g, ln_b, w_att_out, w_conv_out, w_o):
    y_att = att @ w_att_out
    y_conv = conformer_conv(proj[..., GLU0:GA0], conv_w, conv_b, ln_g, ln_b, w_conv_out)
    gate_att = jax.nn.sigmoid(proj[..., GA0:GB0])
    gate_conv = jax.nn.sigmoid(proj[..., GB0:D_IN])
    return (gate_att * y_att + gate_conv * y_conv) @ w_o


def hierarchical_moe(h, w_rg, b_rg, w_re, b_re, w_g, w_u, w_d):
    T, D = h.shape
    hf = h.astype(jnp.float32)
    g_logits = hf @ w_rg.astype(jnp.float32) + b_rg.astype(jnp.float32)
    g_idx = jnp.argmax(g_logits, axis=-1)
    p_group = jnp.take_along_axis(jax.nn.softmax(g_logits, axis=-1), g_idx[:, None], axis=-1)
    e_logits = (hf @ w_re.astype(jnp.float32) + b_re.astype(jnp.float32)).reshape(T, N_GROUPS, EXPERTS_PER_GROUP)
    e_sel = jnp.take_along_axis(e_logits, g_idx[:, None, None], axis=1)[:, 0]
    top_v, top_i = lax.top_k(e_sel, TOP_K_IN_GROUP)
    gate = p_group * jax.nn.softmax(top_v, axis=-1)
    flat_e = (g_idx[:, None] * EXPERTS_PER_GROUP + top_i).reshape(-1)
    flat_tok = jnp.repeat(jnp.arange(T), TOP_K_IN_GROUP)
    flat_w = gate.reshape(-1)
    A = T * TOP_K_IN_GROUP
    order = jnp.argsort(flat_e)
    e_s, tok_s, w_s = flat_e[order], flat_tok[order], flat_w[order]
    counts = jnp.bincount(flat_e, length=N_EXPERTS)
    padded = (counts + MOE_BLOCK - 1) // MOE_BLOCK * MOE_BLOCK
    pad_end = jnp.cumsum(padded)
    pad_start = pad_end - padded
    start = jnp.cumsum(counts) - counts
    dest = pad_start[e_s] + (jnp.arange(A) - start[e_s])
    n_blocks = -(-A // MOE_BLOCK) + N_EXPERTS
    P = n_blocks * MOE_BLOCK
    row_tok = jnp.zeros((P,), jnp.int32).at[dest].set(tok_s.astype(jnp.int32))
    row_w = jnp.zeros((P,), jnp.float32).at[dest].set(w_s)
    block_e = jnp.clip(jnp.searchsorted(pad_end, jnp.arange(n_blocks) * MOE_BLOCK, side='right'), 0, N_EXPERTS - 1)

    def run_block(args):
        tok, e, wt = args
        xb = h[tok]
        y = (jax.nn.silu(xb @ w_g[e]) * (xb @ w_u[e])) @ w_d[e]
        return y.astype(jnp.float32) * wt[:, None]

    ys = lax.map(run_block, (row_tok.reshape(n_blocks, MOE_BLOCK), block_e, row_w.reshape(n_blocks, MOE_BLOCK)))
    out = jnp.zeros((T, D), jnp.float32).at[row_tok].add(ys.reshape(P, D))
    return out.astype(h.dtype)


def setup_inputs(seed: int = 0) -> dict:
    key = jax.random.key(seed)
    ks = jax.random.split(key, 25)
    L, D = DEPTH, D_MODEL

    def nrm(k, shape, s):
        return jax.random.normal(k, shape, jnp.float32) * s

    return {
        "x": nrm(ks[0], (BATCH, SEQ, D), 1.0),
        "c": nrm(ks[1], (BATCH, D), 1.0),
        "ctx": nrm(ks[2], (BATCH, CTX_LEN, D), 1.0),
        "c_ctx": nrm(ks[3], (D,), 1.0),
        "w_mod": nrm(ks[4], (L, D, 6 * D), 0.5 * D ** -0.5),
        "b_mod": nrm(ks[5], (L, 6 * D), 0.01),
        "norm_mix": 1.0 + nrm(ks[6], (L, D), 0.02),
        "w_in": nrm(ks[7], (L, D, D_IN), D ** -0.5),
        "rpb": nrm(ks[8], (L, N_HEADS, 2 * WIN_ROWS_MAX - 1, 2 * WIN_COLS - 1), 0.1),
        "w_att_out": nrm(ks[9], (L, D_ATT, D), D_ATT ** -0.5),
        "conv_w": nrm(ks[10], (L, CONV_WIDTH, D_CONV), CONV_WIDTH ** -0.5),
        "conv_b": nrm(ks[11], (L, D_CONV), 0.01),
        "conv_ln_g": 1.0 + nrm(ks[12], (L, D_CONV), 0.02),
        "conv_ln_b": nrm(ks[13], (L, D_CONV), 0.01),
        "w_conv_out": nrm(ks[14], (L, D_CONV, D), D_CONV ** -0.5),
        "w_o": nrm(ks[15], (L, D, D), D ** -0.5),
        "norm_ffn": 1.0 + nrm(ks[16], (L, D), 0.02),
        "w_router_group": nrm(ks[17], (L, D, N_GROUPS), D ** -0.5),
        "b_router_group": nrm(ks[18], (L, N_GROUPS), 0.01),
        "w_router_expert": nrm(ks[19], (L, D, N_EXPERTS), D ** -0.5),
        "b_router_expert": nrm(ks[20], (L, N_EXPERTS), 0.01),
        "w_exp_gate": nrm(ks[21], (L, N_EXPERTS, D, D_EXPERT), D ** -0.5),
        "w_exp_up": nrm(ks[22], (L, N_EXPERTS, D, D_EXPERT), D ** -0.5),
        "w_exp_down": nrm(ks[23], (L, N_EXPERTS, D_EXPERT, D), D_EXPERT ** -0.5),
        "final_norm": 1.0 + nrm(ks[24], (D,), 0.02),
    }


def reference(x, c, ctx, c_ctx, w_mod, b_mod, norm_mix, w_in, rpb, w_att_out, conv_w, conv_b,
              conv_ln_g, conv_ln_b, w_conv_out, w_o, norm_ffn, w_router_group, b_router_group,
              w_router_expert, b_router_expert, w_exp_gate, w_exp_up, w_exp_down, final_norm):
    B, N, D = x.shape
    n_ctx = ctx.shape[1]
    for l in range(DEPTH):
        last = l == DEPTH - 1
        m_lat = jax.nn.silu(c) @ w_mod[l] + b_mod[l]
        sh1, sc1, g1, sh2, sc2, g2 = jnp.split(m_lat[:, None, :], 6, axis=-1)
        m_ctx = jax.nn.silu(c_ctx) @ w_mod[l] + b_mod[l]
        sh1c, sc1c, g1c, sh2c, sc2c, g2c = jnp.split(m_ctx, 6, axis=-1)

        h = modulate(rmsnorm(x, norm_mix[l]), sh1, sc1)
        hc = modulate(rmsnorm(ctx, norm_mix[l]), sh1c, sc1c)
        proj = h @ w_in[l]
        q = split_heads(proj[..., Q0:K0])
        k = split_heads(proj[..., K0:V0])
        v = split_heads(proj[..., V0:GLU0])
        if last:
            kv_c = hc @ w_in[l][:, K0:GLU0]
            proj_c = None
        else:
            proj_c = hc @ w_in[l]
            kv_c = proj_c[..., K0:GLU0]
        k_c = split_heads(kv_c[..., :D_ATT]).transpose(0, 2, 1, 3)
        v_c = split_heads(kv_c[..., D_ATT:]).transpose(0, 2, 1, 3)
        att = latent_neighbourhood_attention(q, k, v, k_c, v_c, rpb[l])
        y = merge_branches(proj, att, conv_w[l], conv_b[l], conv_ln_g[l], conv_ln_b[l],
                           w_att_out[l], w_conv_out[l], w_o[l])
        x_mid = x + g1 * y
        h2 = modulate(rmsnorm(x_mid, norm_ffn[l]), sh2, sc2)

        if last:
            tokens = h2.reshape(B * N, D)
        else:
            q_c = split_heads(proj_c[..., Q0:K0]).transpose(0, 2, 1, 3)
            att_c = context_attention(q_c, k_c, v_c)
            y_c = merge_branches(proj_c, att_c, conv_w[l], conv_b[l], conv_ln_g[l], conv_ln_b[l],
                                 w_att_out[l], w_conv_out[l], w_o[l])
            ctx_mid = ctx + g1c * y_c
            h2c = modulate(rmsnorm(ctx_mid, norm_ffn[l]), sh2c, sc2c)
            tokens = jnp.concatenate([h2.reshape(B * N, D), h2c.reshape(B * n_ctx, D)], axis=0)
        f = hierarchical_moe(tokens, w_router_group[l], b_router_group[l], w_router_expert[l],
                             b_router_expert[l], w_exp_gate[l], w_exp_up[l], w_exp_down[l])
        x = x_mid + g2 * f[:B * N].reshape(B, N, D)
        if not last:
            ctx = ctx_mid + g2c * f[B * N:].reshape(B, n_ctx, D)
    return rmsnorm(x, final_norm)
```

```python
from contextlib import ExitStack
import numpy as np
import concourse.bass as bass
import concourse.mybir as mybir
from concourse.bass_utils import run_bass_kernel_spmd

F32 = mybir.dt.float32
BF16 = mybir.dt.bfloat16
I32 = mybir.dt.int32
AF = mybir.ActivationFunctionType
ALU = mybir.AluOpType
AX = mybir.AxisListType

D = 1024
SEQ = 2048
NB = 4
NCTX = 256
DIN = 4608
Q0, K0, V0, GLU0, GA0, GB0 = 0, 512, 1024, 1536, 2560, 3584
NE = 32
CAP = 1536
NSLOT = NE * CAP
NTOK = NB * SEQ
EPS = 1e-6
NEG = -30000.0
BIG = 1.0e9

SEM_LIMIT = 12000
ENGS = ("pe", "act", "dve", "pool", "sp")


class T:
    __slots__ = ("name", "w", "r", "excl")

    def __init__(self, name="t", excl=False):
        self.name = name
        self.w = []
        self.r = []
        self.excl = excl


class Prog:
    def __init__(self, nc, es, dma_ring=6):
        self.nc = nc
        self.es = es
        self.q = {e: [] for e in ENGS}
        self.sem = {}
        self.cnt = {}
        self.known = {e: {} for e in ENGS}
        self.nsem = 0
        for e in ENGS:
            self.sem[e] = self._newsem(e)
            self.cnt[e] = 0
        self.ring = {e: [[self._newsem("d" + e), 0] for _ in range(dma_ring)] for e in ("sp", "act", "pool")}
        self.ring_i = {e: 0 for e in ("sp", "act", "pool")}
        self.cbase = None
        self.cond = None
        self.cknown = {e: {} for e in ENGS}
        self.cnt_ap = None
        self.cnt_event = None
        self.regs = {}
        self.loaded = {}

    def set_cond(self, cond):
        if cond != self.cond:
            self.cond = cond
            self.cknown = {e: {} for e in ENGS}
            self.cbase = {id(self.sem[e]): self.cnt[e] for e in ENGS}
            for ring in self.ring.values():
                for slot in ring:
                    self.cbase[id(slot[0])] = slot[1]

    def _newsem(self, tag):
        self.nsem += 1
        return self.es.enter_context(self.nc.semaphore("s%s_%d" % (tag, self.nsem)))

    def _need(self, eng, ev, need, war=False):
        sem, val, src = ev
        if src == eng and eng == "pe":
            return
        if self.known[eng].get(id(sem), -1) >= val:
            return
        if self.cond is not None and self.cknown[eng].get(id(sem), -1) >= val:
            return
        if src is not None and sem is self.sem[src] and val > self.cnt[src]:
            raise RuntimeError("wait on an event whose incrementing instruction is not recorded yet (deadlock risk): %s -> %s" % (src, eng))
        cur = need.get(id(sem))
        if cur is None or cur[1] < val:
            need[id(sem)] = (sem, val)

    def _deps(self, eng, reads, writes, is_dma=False):
        need = {}
        for t in reads:
            for ev in t.w:
                self._need(eng, ev, need)
            if t.excl:
                for ev in t.r:
                    self._need(eng, ev, need, war=True)
        for t in writes:
            for ev in t.w:
                if is_dma and ev[2] is None and not t.r:
                    continue
                self._need(eng, ev, need)
            for ev in t.r:
                self._need(eng, ev, need, war=True)
        waits = []
        for k, (sem, val) in need.items():
            self._learn(eng, k, val)
            waits.append((sem, val))
        return waits

    def _learn(self, eng, k, val):
        if self.cond is None:
            self.known[eng][k] = val
        else:
            self.cknown[eng][k] = val

    def _mark(self, ev, reads, writes):
        for t in reads:
            if ev[2] is not None:
                t.r = [x for x in t.r if x[2] != ev[2]]
            t.r.append(ev)
        for t in writes:
            if ev[2] is None and not t.r:
                t.w = [x for x in t.w if x[2] is None] + [ev]
            else:
                t.w = [ev]
            t.r = []

    def op(self, eng, fn, reads=(), writes=(), inc=True):
        waits = self._deps(eng, reads, writes)
        ev = (self.sem[eng], self.cnt[eng] + 1, eng)
        self.q[eng].append((waits, fn, self.sem[eng] if inc else None, 1, self.cond, self.cbase if self.cond is not None else None))
        self._mark(ev, reads, writes)
        if inc:
            self.cnt[eng] += 1
            if self.cnt[eng] >= SEM_LIMIT:
                self.sem[eng] = self._newsem(eng)
                self.cnt[eng] = 0

    def dma(self, eng, fn, reads=(), writes=()):
        waits = self._deps(eng, reads, writes, is_dma=True)
        ring = self.ring[eng]
        i = self.ring_i[eng]
        self.ring_i[eng] = (i + 1) % len(ring)
        slot = ring[i]
        if slot[1] > 0 and max(self.known[eng].get(id(slot[0]), -1), self.cknown[eng].get(id(slot[0]), -1) if self.cond is not None else -1) < slot[1]:
            self._learn(eng, id(slot[0]), slot[1])
            waits.append((slot[0], slot[1]))
        if slot[1] >= SEM_LIMIT * 4:
            slot[0] = self._newsem("d" + eng)
            slot[1] = 0
        slot[1] += 16
        ev = (slot[0], slot[1], None)
        self.q[eng].append((waits, fn, slot[0], 16, self.cond, self.cbase if self.cond is not None else None))
        self._mark(ev, reads, writes)
        return ev

    def barrier(self):
        evs = []
        for e in ENGS:
            if self.cnt[e] > 0:
                evs.append((self.sem[e], self.cnt[e], e))
        for ring in self.ring.values():
            for slot in ring:
                if slot[1] > 0:
                    evs.append((slot[0], slot[1], None))
        for e in ENGS:
            need = {}
            for ev in evs:
                self._need(e, ev, need)
            waits = []
            for k, (sem, val) in need.items():
                self.known[e][k] = val
                waits.append((sem, val))
            if waits:
                self.q[e].append((waits, None, None, 0, None, 0))

    def flush(self):
        self.barrier()
        self.emit()

    def emit(self):
        q = self.q
        self.q = {e: [] for e in ENGS}
        if not any(q.values()):
            return

        def run(e, seg):
            for waits, fn, sem, n, _c, _v in seg:
                for (s_, v) in waits:
                    e.wait_ge(s_, v)
                if fn is None:
                    continue
                ins = fn(e)
                if sem is not None:
                    ins.then_inc(sem, n)

        def replay(e, lst, name=None):
            i = 0
            while i < len(lst):
                cond = lst[i][4]
                j = i
                while j < len(lst) and lst[j][4] == cond:
                    j += 1
                seg = lst[i:j]
                i = j
                if cond is None:
                    run(e, seg)
                    continue
                ex, thr = cond
                key = (name, ex % 2)
                if key not in self.regs:
                    self.regs[key] = e.alloc_register("cnt_%s_%d" % key)
                reg = self.regs[key]
                if self.loaded.get(key) != ex:
                    if (name, "ev") not in self.loaded:
                        e.wait_ge(self.cnt_event[0], self.cnt_event[1])
                        self.loaded[(name, "ev")] = True
                    e.reg_load(reg, self.cnt_ap[0:1, ex:ex + 1])
                    self.loaded[key] = ex
                tot = {}
                first = seg[0][5]
                for waits, fn, sem, n, _c, va in seg:
                    if fn is not None and sem is not None:
                        tot[id(sem)] = (sem, tot.get(id(sem), (sem, 0))[1] + n)
                if tot:
                    with e.If_lt(reg, thr + 1):
                        for waits, fn, sem, n, _c, va in seg:
                            for (s_, v) in waits:
                                if id(s_) in first and v > first[id(s_)]:
                                    continue
                                e.wait_ge(s_, v)
                        e.drain()
                        for sem, n in tot.values():
                            e.sem_inc(sem, n)
                    with e.Else():
                        run(e, seg)
                else:
                    with e.If_lt(reg, thr + 1):
                        e.nop()
                    with e.Else():
                        run(e, seg)

        with self.nc.Block() as block:
            @block.tensor
            def _(e):
                replay(e, q["pe"], "pe")

            @block.scalar
            def _(e):
                replay(e, q["act"], "act")

            @block.vector
            def _(e):
                replay(e, q["dve"], "dve")

            @block.gpsimd
            def _(e):
                replay(e, q["pool"], "pool")

            @block.sync
            def _(e):
                replay(e, q["sp"], "sp")


def r_start(r):
    return min(max(r - 4, 0), 24)


def build(stop=99, dbg=False, cap=CAP):
    nslot = NE * cap
    nc = bass.Bass("TRN2", target_bir_lowering=False)

    def din(name, shape, dt=F32):
        return nc.dram_tensor(name, list(shape), dt, kind="ExternalInput").ap()

    xT = din("xT", [NB, 8, 128, SEQ])
    ctxT = din("ctxT", [NB, 8, 128, NCTX])
    cT = din("cT", [128, 8 * 5])
    w_mod = din("w_mod", [D, 6 * D])
    b_modT = din("b_modT", [128, 48])
    nmixT = din("nmixT", [128, 8])
    nffnT = din("nffnT", [128, 8])
    w_in = din("w_in", [D, DIN])
    tt_in = din("tt", [128, 4 * 15 * 64])
    w_ao = din("w_att_out", [512, D])
    w_co = din("w_conv_out", [512, D])
    w_o = din("w_o", [D, D])
    cwT = din("cwT", [128, 4 * 31])
    cvec = din("cvec", [128, 12])
    wr_in = din("wr", [128, 8 * 36])
    br_in = din("br", [128, 4 * 36])
    ecap_in = din("ecap", [128, 4 * 32])
    w_g = din("w_exp_gate", [NE, D, 512])
    w_u = din("w_exp_up", [NE, D, 512])
    w_d = din("w_exp_down", [NE, 512, D])
    fnorm = din("fnorm", [128, D])
    out = nc.dram_tensor("out", [NTOK, D], F32, kind="ExternalOutput").ap()
    Xs = nc.dram_tensor("Xs", [nslot + 2 * NTOK, D], BF16).ap()
    Ys = nc.dram_tensor("Ys", [nslot + 2 * NTOK, D], F32).ap()
    xmid = nc.dram_tensor("xmid", [NTOK, D], F32).ap()
    wgs_d = nc.dram_tensor("wgs_d", [8, 128, 2 * 8 * 128], BF16).ap()
    cnt_d = nc.dram_tensor("cnt_d", [1, NE], I32).ap()
    dbg_out = {}

    def dout(name, shape, dt=F32):
        a = nc.dram_tensor(name, list(shape), dt, kind="ExternalOutput").ap()
        dbg_out[name] = a
        return a

    with ExitStack() as es:
        P = Prog(nc, es)

        uid = [0]

        def sbt(scope, name, shape, dt):
            uid[0] += 1
            return scope.enter_context(nc.sbuf_tensor("sb%d_%s" % (uid[0], name), list(shape), dt))

        def mm(o, lhsT, rhs, start, stop_, reads, wt, inc):
            P.op("pe", lambda e: e.matmul(o, lhsT, rhs, start=start, stop=stop_, skip_group_check=True),
                 reads=reads, writes=[wt], inc=inc)

        def tr(o, in_, ident, reads, wt, inc=True):
            P.op("pe", lambda e: e.transpose(o, in_, ident), reads=reads, writes=[wt], inc=inc)

        def act(o, in_, func, reads, writes, bias=None, scale=None, accum=None):
            kw = {}
            if bias is not None:
                kw["bias"] = bias
            if scale is not None:
                kw["scale"] = scale
            if accum is not None:
                kw["accum_out"] = accum
            P.op("act", lambda e: e.activation(out=o, in_=in_, func=func, **kw), reads=reads, writes=writes)

        def tt_(eng, o, a, b, op, reads, writes):
            P.op(eng, lambda e: e.tensor_tensor(out=o, in0=a, in1=b, op=op), reads=reads, writes=writes)

        def ts_(eng, o, a, s1, s2, op0, op1, reads, writes):
            if s2 is None:
                P.op(eng, lambda e: e.tensor_scalar(out=o, in0=a, scalar1=s1, scalar2=None, op0=op0), reads=reads, writes=writes)
            else:
                P.op(eng, lambda e: e.tensor_scalar(out=o, in0=a, scalar1=s1, scalar2=s2, op0=op0, op1=op1), reads=reads, writes=writes)

        def stt_(eng, o, a, s, b, op0, op1, reads, writes):
            P.op(eng, lambda e: e.scalar_tensor_tensor(out=o, in0=a, scalar=s, in1=b, op0=op0, op1=op1), reads=reads, writes=writes)

        def cp(eng, o, a, reads, writes):
            if eng == "act":
                P.op("act", lambda e: e.copy(out=o, in_=a), reads=reads, writes=writes)
            else:
                P.op(eng, lambda e: e.tensor_copy(out=o, in_=a), reads=reads, writes=writes)

        def red(eng, o, a, op, reads, writes):
            P.op(eng, lambda e: e.tensor_reduce(out=o, in_=a, axis=AX.X, op=op), reads=reads, writes=writes)

        def rsqrt_(o, in_, scale, reads, wt):
            act(o, in_, AF.Sqrt, reads + [Tc], [wt], bias=epsc[:, 0:1], scale=scale)
            P.op("dve", lambda e: e.reciprocal(out=o, in_=o), reads=[wt], writes=[wt])

        def mset(eng, o, v, writes):
            P.op(eng, lambda e: e.memset(o, v), writes=writes)

        def ld(q, o, in_, writes, reads=()):
            P.dma(q, lambda e: e.dma_start(out=o, in_=in_), reads=reads, writes=writes)

        def dump(name, src_ap, shape, t, dt=F32):
            if not dbg:
                return
            d = dout(name, shape, dt)
            ld("sp", d, src_ap, writes=[T()], reads=[t])

        NBANK = 7
        ps_es = ExitStack()
        banks = [ps_es.enter_context(nc.psum_tensor("pb%d" % i, [128, 512], F32)) for i in range(NBANK)]
        bankT = [T("pb%d" % i, excl=True) for i in range(NBANK)]
        pbf = ps_es.enter_context(nc.psum_tensor("pbf", [128, 1024], BF16))
        Tpbf = T("pbf", excl=True)
        gen = {"list": [0, 1, 2, 3, 4], "i": 0, "banks": banks, "bankT": bankT}

        def bank():
            i = gen["list"][gen["i"] % len(gen["list"])]
            gen["i"] += 1
            return gen["banks"][i], gen["bankT"][i]

        cs = es
        ident_f = sbt(cs, "ident_f", [128, 128], F32)
        ident_b = sbt(cs, "ident_b", [128, 128], BF16)
        ones_b = sbt(cs, "ones_b", [128, 128], BF16)
        ones_f = sbt(cs, "ones_f", [128, 128], F32)
        shift_b = sbt(cs, "shift_b", [128, 128], BF16)
        onesbd = sbt(cs, "onesbd", [128, 128], BF16)
        onespad = sbt(cs, "onespad", [128, 2, 128], BF16)
        utri_f = sbt(cs, "utri_f", [128, 128], F32)
        iot = sbt(cs, "iot", [128, 128], F32)
        epsc = sbt(cs, "epsc", [128, 1], F32)
        tmpc = sbt(cs, "tmpc", [128, 128], F32)
        TTb = sbt(cs, "TTb", [128, 4, 15 * 64], BF16)
        bmod = sbt(cs, "bmod", [128, 48], F32)
        nmix = sbt(cs, "nmix", [128, 8], F32)
        nffn = sbt(cs, "nffn", [128, 8], F32)
        cw = sbt(cs, "cw", [128, 4, 31], F32)
        cv = sbt(cs, "cv", [128, 3, 4], F32)
        wr = sbt(cs, "wr", [128, 8, 36], F32)
        br = sbt(cs, "br", [128, 4, 36], F32)
        wrh = sbt(cs, "wrh", [128, 8, 36], BF16)
        wrl = sbt(cs, "wrl", [128, 8, 36], BF16)
        utri_b = sbt(cs, "utri_b", [128, 128], BF16)
        ecap = sbt(cs, "ecap", [128, 4, 32], F32)
        mod = sbt(cs, "mod", [128, 48, 5], F32)
        A1 = sbt(cs, "A1", [128, 8, 5], F32)
        A2 = sbt(cs, "A2", [128, 8, 5], F32)
        Acc = sbt(cs, "Acc", [128, 32], F32)
        Wk = sbt(cs, "Wk", [128, 64, 2], F32)
        Dk = sbt(cs, "Dk", [128, 64, 2], I32)
        Tc = T("consts")
        Tmod = T("mod")
        TAcc = T("Acc")
        TWk = T("Wk")
        TDk = T("Dk")
        TXs, TYs, Txmid = T("Xs"), T("Ys"), T("xmid")
        Tout = T("out")
        Twgd = T("wgs_d")

        P.op("pool", lambda e: e.iota(iot[:], pattern=[[1, 128]], base=0, channel_multiplier=-1,
                                      allow_small_or_imprecise_dtypes=True), writes=[Tc])
        ts_("dve", ident_f[:], iot[:], 0.0, None, ALU.is_equal, None, [Tc], [Tc])
        cp("dve", ident_b[:], ident_f[:], [Tc], [Tc])
        ts_("dve", utri_f[:], iot[:], 0.0, None, ALU.is_gt, None, [Tc], [Tc])
        ts_("dve", tmpc[:], iot[:], 64.0, None, ALU.is_equal, None, [Tc], [Tc])
        ts_("dve", ones_f[:], iot[:], -64.0, None, ALU.is_equal, None, [Tc], [Tc])
        tt_("dve", shift_b[:], tmpc[:], ones_f[:], ALU.add, [Tc], [Tc])
        mset("dve", ones_f[:], 1.0, [Tc])
        mset("dve", ones_b[:], 1.0, [Tc])
        mset("dve", onesbd[:], 0.0, [Tc])
        mset("dve", onesbd[0:64, 0:64], 1.0, [Tc])
        mset("dve", onesbd[64:128, 64:128], 1.0, [Tc])
        mset("dve", onespad[:], 0.0, [Tc])
        mset("dve", onespad[:, 0, 0:64], 1.0, [Tc])
        mset("dve", onespad[:, 1, 64:128], 1.0, [Tc])
        mset("dve", Acc[:], 0.0, [TAcc])
        mset("dve", epsc[:], EPS, [Tc])
        ld("pool", TTb[:].rearrange("p a b -> p (a b)"), tt_in[:, :], [Tc])
        ld("sp", bmod[:], b_modT[:, :], [Tc])
        ld("sp", nmix[:], nmixT[:, :], [Tc])
        ld("sp", nffn[:], nffnT[:, :], [Tc])
        ld("sp", cw[:].rearrange("p a b -> p (a b)"), cwT[:, :], [Tc])
        ld("sp", cv[:].rearrange("p a b -> p (a b)"), cvec[:, :], [Tc])
        ld("sp", wr[:].rearrange("p a b -> p (a b)"), wr_in[:, :], [Tc])
        ld("sp", br[:].rearrange("p a b -> p (a b)"), br_in[:, :], [Tc])
        cp("dve", wrh[:], wr[:], [Tc], [Tc])
        tt_("dve", wrl[:], wr[:], wrh[:], ALU.subtract, [Tc], [Tc])
        cp("dve", utri_b[:], utri_f[:], [Tc], [Tc])
        ld("sp", ecap[:].rearrange("p a b -> p (a b)"), ecap_in[:, :], [Tc])

        with ExitStack() as s0:
            cin = sbt(s0, "cin", [128, 8, 5], F32)
            csb = sbt(s0, "csb", [128, 8, 5], BF16)
            wm = [sbt(s0, "wm%d" % i, [128, 8, 512], BF16) for i in range(2)]
            Twm = [T("wm0"), T("wm1")]
            Tcin = T("cin")
            ld("sp", cin[:].rearrange("p a b -> p (a b)"), cT[:, :], [Tcin])
            act(csb[:], cin[:], AF.Silu, [Tcin], [Tcin])
            for j in range(12):
                wb_, tw = wm[j % 2], Twm[j % 2]
                ld("pool", wb_[:], w_mod[:, j * 512:(j + 1) * 512].rearrange("(k p) n -> p k n", p=128), [tw])
                for m in range(4):
                    pb, tb = bank()
                    for k in range(8):
                        mm(pb[:, 0:5], wb_[:, k, m * 128:(m + 1) * 128], csb[:, k, :], k == 0, k == 7, [tw, Tcin], tb, k == 7)
                    ci = j * 4 + m
                    ts_("dve", mod[:, ci, :], pb[:, 0:5], bmod[:, ci:ci + 1], None, ALU.add, None, [tb, Tc], [Tmod])
            ts_("dve", A1[:], mod[:, 8:16, :], 1.0, None, ALU.add, None, [Tmod], [Tmod])
            ts_("dve", A2[:], mod[:, 32:40, :], 1.0, None, ALU.add, None, [Tmod], [Tmod])
            for k in range(8):
                ts_("dve", A1[:, k, :], A1[:, k, :], nmix[:, k:k + 1], None, ALU.mult, None, [Tmod, Tc], [Tmod])
                ts_("dve", A2[:, k, :], A2[:, k, :], nffn[:, k:k + 1], None, ALU.mult, None, [Tmod, Tc], [Tmod])
            dump("d_mod", mod[:].rearrange("p a b -> p (a b)"), [128, 240], Tmod)
            wtmp = [sbt(s0, "wtmp%d" % i, [128, 2, 8, 128], BF16) for i in range(2)]
            Twtmp = [T("wtmp0"), T("wtmp1")]
            for m in range(8):
                wt_, twt = wtmp[m % 2], Twtmp[m % 2]
                ld("pool", wt_[:, 0, :, :], w_in[:, GA0 + m * 128:GA0 + (m + 1) * 128].rearrange("(k p) n -> p k n", p=128), [twt])
                ld("pool", wt_[:, 1, :, :], w_in[:, GB0 + m * 128:GB0 + (m + 1) * 128].rearrange("(k p) n -> p k n", p=128), [twt])
                ld("sp", wgs_d[m], wt_[:].rearrange("p a k n -> p (a k n)"), [Twgd], reads=[twt])
            P.flush()

        def finish():
            P.barrier()
            P.emit()

        if stop <= 0:
            finish()
            return nc, dbg_out

        for b in range(NB):
            with ExitStack() as sq_:
                hT = sbt(sq_, "hT", [128, 8, SEQ], BF16)
                attT = sbt(sq_, "attT", [128, 4, SEQ], BF16)
                cTt = sbt(sq_, "cTt", [128, 4, SEQ], BF16)
                ThT, TattT, TcT = T("hT"), T("attT"), T("cT")
                with ExitStack() as su:
                    uT = sbt(su, "uT", [128, 4, SEQ + 30], BF16)
                    TuT = T("uT")
                    Dg = sbt(su, "Dg", [128, 4, 31, 128], BF16)
                    TDg = T("Dg")
                    for j in range(4):
                        tt_("dve", Dg[:, j, :, :], ident_f[:].unsqueeze(1).to_broadcast([128, 31, 128]),
                            cw[:, j, :].unsqueeze(2).to_broadcast([128, 31, 128]), ALU.mult, [Tc], [TDg])
                    with ExitStack() as s13:
                        hcT = sbt(s13, "hcT", [128, 8, NCTX], BF16)
                        ThcT = T("hcT")
                        xs = [sbt(s13, "xs%d" % i, [128, 8, 256], F32) for i in range(3)]
                        Txs = [T("xs%d" % i) for i in range(3)]
                        sqb = [sbt(s13, "sqb0", [128, 8, 256], BF16)] * 2
                        Tsq = [T("sq0")] * 2
                        rstd = [sbt(s13, "rstd%d" % i, [128, 256], F32) for i in range(2)]
                        Trstd = [T("rstd0"), T("rstd1")]
                        wbuf = [sbt(s13, "wbuf%d" % i, [128, 2, 8, 128], BF16) for i in range(2)]
                        Twb = [T("wb0"), T("wb1")]
                        wqkv = sbt(s13, "wqkv", [128, 3, 8, 128], BF16)
                        Twqkv = T("wqkv")
                        qT = sbt(s13, "qT", [128, SEQ], BF16)
                        Kbd = sbt(s13, "Kbd", [128, 32, 128], BF16)
                        Vbd = sbt(s13, "Vbd", [128, 32, 128], BF16)
                        kc = sbt(s13, "kc", [128, NCTX], BF16)
                        Vcp = sbt(s13, "Vcp", [128, 2, 2, 128], BF16)
                        Vtok = sbt(s13, "Vtok", [128, 512], BF16)
                        TqT, TKbd, TVbd, Tkc, TVcp, TVtok = T("qT"), T("Kbd"), T("Vbd"), T("kc"), T("Vcp"), T("Vtok")
                        Pt = [sbt(s13, "Pt%d" % i, [128, 512], BF16) for i in range(3)]
                        TPt = [T("Pt0"), T("Pt1"), T("Pt2")]
                        rZ = sbt(s13, "rZ", [128, 512], F32)
                        TrZ = T("rZ")
                        sg = [sbt(s13, "sg%d" % i, [128, 512], BF16) for i in range(2)]
                        Tsg = [T("sg0"), T("sg1")]

                        mset("pool", Kbd[:], 0.0, [TKbd])
                        mset("pool", Vbd[:], 0.0, [TVbd])
                        mset("pool", Vcp[:], 0.0, [TVcp])
                        mset("pool", uT[:, :, 0:15], 0.0, [TuT])
                        mset("pool", uT[:, :, SEQ + 15:SEQ + 30], 0.0, [TuT])

                        NT_ = 256
                        jobs = [(ctxT[b], 0, hcT, ThcT, 4)] + [(xT[b], t_ * NT_, hT, ThT, b) for t_ in range(SEQ // NT_)]
                        nj_ = len(jobs)
                        stb = {}

                        def nm_load(it):
                            src, n0, dst, Tdst, bcol = jobs[it]
                            ld("sp", xs[it % 3][:], src[:, :, n0:n0 + NT_].rearrange("k p n -> p k n"), [Txs[it % 3]])

                        def nm_sq(it):
                            x_, tx = xs[it % 3], Txs[it % 3]
                            tt_("dve", sqb[it % 2][:], x_[:], x_[:], ALU.mult, [tx], [Tsq[it % 2]])
                            pb, tb = bank()
                            stb[it] = (pb, tb)
                            for k in range(8):
                                mm(pb[:, 0:NT_], ones_b[:], sqb[it % 2][:, k, :], k == 0, k == 7, [Tsq[it % 2], Tc], tb, k == 7)

                        def nm_rs(it):
                            pb, tb = stb[it]
                            rsqrt_(rstd[it % 2][:], pb[:, 0:NT_], 1.0 / D, [tb], Trstd[it % 2])

                        def nm_B(it):
                            src, n0, dst, Tdst, bcol = jobs[it]
                            x_, tx = xs[it % 3], Txs[it % 3]
                            tt_("dve", x_[:], x_[:], rstd[it % 2][:].unsqueeze(1).to_broadcast([128, 8, NT_]), ALU.mult, [tx, Trstd[it % 2]], [tx])
                            for k in range(8):
                                act(dst[:, k, n0:n0 + NT_], x_[:, k, :], AF.Identity, [tx, Tmod], [Tdst],
                                    bias=mod[:, k, bcol:bcol + 1], scale=A1[:, k, bcol:bcol + 1])

                        nm_load(0)
                        nm_load(1)
                        nm_sq(0)
                        nm_sq(1)
                        nm_rs(0)
                        for it_ in range(nj_):
                            if it_ + 2 < nj_:
                                nm_load(it_ + 2)
                            if it_ + 1 < nj_:
                                nm_rs(it_ + 1)
                            nm_B(it_)
                            if it_ + 2 < nj_:
                                nm_sq(it_ + 2)
                        if b == 0:
                            dump("d_hT", hT[:].rearrange("p a b -> p (a b)"), [128, 8 * SEQ], ThT, BF16)
                            dump("d_hcT", hcT[:].rearrange("p a b -> p (a b)"), [128, 8 * NCTX], ThcT, BF16)
                        if stop <= 1:
                            P.barrier()
                            break

                        it = 0
                        for j in range(4):
                            wgl, twgl = wbuf[j % 2], Twb[j % 2]
                            ld("pool", wgl[:, 0, :, :], w_in[:, GLU0 + j * 128:GLU0 + (j + 1) * 128].rearrange("(k p) n -> p k n", p=128), [twgl])
                            ld("pool", wgl[:, 1, :, :], w_in[:, GLU0 + 512 + j * 128:GLU0 + 512 + (j + 1) * 128].rearrange("(k p) n -> p k n", p=128), [twgl])
                            for t in range(4):
                                pa, ta = bank()
                                pg, tg = bank()
                                for k in range(8):
                                    mm(pa[:], wgl[:, 0, k, :], hT[:, k, t * 512:(t + 1) * 512], k == 0, k == 7, [twgl, ThT], ta, k == 7)
                                for k in range(8):
                                    mm(pg[:], wgl[:, 1, k, :], hT[:, k, t * 512:(t + 1) * 512], k == 0, k == 7, [twgl, ThT], tg, k == 7)
                                s_, tsg = sg[it % 2], Tsg[it % 2]
                                it += 1
                                act(s_[:], pg[:], AF.Sigmoid, [tg], [tsg])
                                tt_("dve", uT[:, j, 15 + t * 512:15 + (t + 1) * 512], pa[:], s_[:], ALU.mult, [ta, tsg], [TuT])
                        if b == 0:
                            dump("d_uT", uT[:].rearrange("p a b -> p (a b)"), [128, 4 * (SEQ + 30)], TuT, BF16)
                        if stop <= 2:
                            P.barrier()
                            break

                        gen["list"] = [0, 1, 2]
                        for hp in range(4):
                            for i, c0 in enumerate((Q0, K0, V0)):
                                ld("pool", wqkv[:, i, :, :], w_in[:, c0 + hp * 128:c0 + (hp + 1) * 128].rearrange("(k p) n -> p k n", p=128), [Twqkv])
                            pb, tb = bank()
                            for k in range(8):
                                mm(pb[:, 0:NCTX], wqkv[:, 1, k, :], hcT[:, k, :], k == 0, k == 7, [Twqkv, ThcT], tb, k == 7)
                            cp("act", kc[:], pb[:, 0:NCTX], [tb], [Tkc])
                            pb, tb = bank()
                            for cc in range(2):
                                for k in range(8):
                                    mm(pb[:, cc * 128:(cc + 1) * 128], hcT[:, k, cc * 128:(cc + 1) * 128], wqkv[:, 2, k, :], k == 0, k == 7, [Twqkv, ThcT], tb, k == 7)
                            pv3 = pb[:, 0:256].rearrange("p (c f) -> p c f", c=2)
                            cp("dve", Vcp[:, :, 0, 0:64], pv3[:, :, 0:64], [tb], [TVcp])
                            cp("dve", Vcp[:, :, 1, 64:128], pv3[:, :, 64:128], [tb], [TVcp])
                            if stop <= 2.1:
                                continue
                            for t in range(4):
                                pb, tb = bank()
                                for k in range(8):
                                    mm(pb[:], wqkv[:, 0, k, :], hT[:, k, t * 512:(t + 1) * 512], k == 0, k == 7, [Twqkv, ThT], tb, k == 7)
                                act(qT[:, t * 512:(t + 1) * 512], pb[:], AF.Copy, [tb], [TqT], scale=0.125)
                                pb, tb = bank()
                                for k in range(8):
                                    mm(pb[:], wqkv[:, 1, k, :], hT[:, k, t * 512:(t + 1) * 512], k == 0, k == 7, [Twqkv, ThT], tb, k == 7)
                                cp("dve", Kbd[0:64, t * 8:(t + 1) * 8, 0:64], pb[0:64, :].rearrange("p (r c) -> p r c", r=8), [tb], [TKbd])
                                cp("act", Kbd[64:128, t * 8:(t + 1) * 8, 64:128], pb[64:128, :].rearrange("p (r c) -> p r c", r=8), [tb], [TKbd])
                            if stop <= 2.2:
                                continue
                            for g4 in range(4):
                                pv, tv = bank()
                                for tq in range(4):
                                    tt16 = g4 * 4 + tq
                                    for k in range(8):
                                        mm(pv[:, tq * 128:(tq + 1) * 128], hT[:, k, tt16 * 128:(tt16 + 1) * 128], wqkv[:, 2, k, :], k == 0, k == 7, [Twqkv, ThT], tv, k == 7)
                                cp("act", Vtok[:], pv[:], [tv], [TVtok])
                                pw, tw = bank()
                                mm(pw[:], shift_b[:], Vtok[:], True, True, [TVtok, Tc], tw, True)
                                r0 = g4 * 8
                                vv = Vbd[:, r0:r0 + 8, :].rearrange("p (t two) c -> p t two c", two=2)
                                pv3 = pv[:].rearrange("p (t c) -> p t c", t=4)
                                pw3 = pw[:].rearrange("p (t c) -> p t c", t=4)
                                vt3 = Vtok[:].rearrange("p (t c) -> p t c", t=4)
                                cp("pool", vv[0:64, :, 0, 0:64], vt3[0:64, :, 0:64], [TVtok], [TVbd])
                                cp("pool", vv[64:128, :, 1, 64:128], vt3[64:128, :, 64:128], [TVtok], [TVbd])
                                cp("dve", vv[64:128, :, 0, 64:128], pw3[64:128, :, 64:128], [tw], [TVbd])
                                cp("dve", vv[0:64, :, 1, 0:64], pw3[0:64, :, 0:64], [tw], [TVbd])
                            if b == 0 and hp == 0:
                                dump("d_qT", qT[:], [128, SEQ], TqT, BF16)
                                dump("d_Kbd", Kbd[:].rearrange("p a b -> p (a b)"), [128, 32 * 128], TKbd, BF16)
                                dump("d_Vbd", Vbd[:].rearrange("p a b -> p (a b)"), [128, 32 * 128], TVbd, BF16)
                                dump("d_kc", kc[:], [128, NCTX], Tkc, BF16)
                                dump("d_Vcp", Vcp[:].rearrange("p a b c -> p (a b c)"), [128, 512], TVcp, BF16)
                            if stop <= 3:
                                continue
                            steps = []
                            for c in range(4):
                                q0c = c * 512
                                oz = ((banks[3], bankT[3], banks[4], bankT[4]), (banks[5], bankT[5], banks[6], bankT[6]))[c % 2]
                                first = True
                                for hh in range(2):
                                    for cc in range(2):
                                        steps.append(dict(kind="ctx", hh=hh, cc=cc, n=512, q0=q0c, oa=0, oz=oz, first=first, last=False, c=c))
                                        first = False
                                rows = range(c * 8, c * 8 + 8)
                                krows = []
                                for rk in range(32):
                                    val = [r for r in rows if r_start(r) <= rk <= r_start(r) + 7]
                                    if val:
                                        krows.append((rk, val[0], val[-1]))
                                for idx_, (rk, lo, hi) in enumerate(krows):
                                    steps.append(dict(kind="loc", rk=rk, n=(hi - lo + 1) * 64, q0=lo * 64, oa=lo * 64 - q0c, j0=7 + lo - rk, oz=oz,
                                                      first=False, last=idx_ == len(krows) - 1, c=c))
                            sbanks = [(banks[i_], bankT[i_]) for i_ in (0, 1, 2)]

                            def emit_S(i_):
                                st = steps[i_]
                                ps_, tps = sbanks[i_ % 3]
                                n = st["n"]
                                if st["kind"] == "ctx":
                                    hh, cc = st["hh"], st["cc"]
                                    mm(ps_[:], kc[hh * 64:(hh + 1) * 64, cc * 128:(cc + 1) * 128], qT[hh * 64:(hh + 1) * 64, st["q0"]:st["q0"] + 512],
                                       True, True, [Tkc, TqT], tps, True)
                                else:
                                    mm(ps_[:, 0:n], Kbd[:, st["rk"], :], qT[:, st["q0"]:st["q0"] + n], True, False, [TKbd, TqT], tps, False)
                                    mm(ps_[:, 0:n], ident_b[:], TTb[:, hp, st["j0"] * 64:st["j0"] * 64 + n], False, True, [Tc], tps, True)

                            def emit_E(i_):
                                st = steps[i_]
                                ps_, tps = sbanks[i_ % 3]
                                n = st["n"]
                                act(Pt[i_ % 3][:, 0:n], ps_[:, 0:n], AF.Exp, [tps], [TPt[i_ % 3]])

                            def emit_PV(i_):
                                st = steps[i_]
                                n = st["n"]
                                p_, tp = Pt[i_ % 3], TPt[i_ % 3]
                                Oa, TO, Za, TZ = st["oz"]
                                oa = st["oa"]
                                if st["kind"] == "ctx":
                                    mm(Oa[:], Vcp[:, st["cc"], st["hh"], :], p_[:], st["first"], False, [TVcp, tp], TO, False)
                                    mm(Za[:], onespad[:, st["hh"], :], p_[:], st["first"], False, [Tc, tp], TZ, True)
                                else:
                                    mm(Oa[:, oa:oa + n], Vbd[:, st["rk"], :], p_[:, 0:n], False, st["last"], [TVbd, tp], TO, False)
                                    mm(Za[:, oa:oa + n], onesbd[:], p_[:, 0:n], False, st["last"], [Tc, tp], TZ, True)
                                if st["last"]:
                                    q0c_ = st["c"] * 512
                                    P.op("dve", lambda e: e.reciprocal(out=rZ[:], in_=Za[:]), reads=[TZ], writes=[TrZ])
                                    tt_("dve", attT[:, hp, q0c_:q0c_ + 512], Oa[:], rZ[:], ALU.mult, [TO, TZ, TrZ], [TattT])

                            ns_ = len(steps)
                            emit_S(0)
                            emit_S(1)
                            for i_ in range(ns_):
                                emit_E(i_)
                                if i_ + 2 < ns_:
                                    emit_S(i_ + 2)
                                emit_PV(i_)
                        gen["list"] = [0, 1, 2, 3, 4]
                        if b == 0:
                            dump("d_attT", attT[:].rearrange("p a b -> p (a b)"), [128, 4 * SEQ], TattT, BF16)
                        xs1b = xs[1][:].rearrange("p k n -> p (k n)").bitcast(BF16).rearrange("p (a n) -> p a n", a=8)
                        yf = xs[0][:].rearrange("p k n -> p (k n)").rearrange("p (a n) -> p a n", a=4)
                        yb = xs1b[:, 0:4, :]
                        ysq = xs1b[:, 4:8, :]
                        mu = xs[2][:, 0:2, :].rearrange("p a n -> p (a n)")
                        msq = xs[2][:, 2:4, :].rearrange("p a n -> p (a n)")
                        rsc = xs[2][:, 4:6, :].rearrange("p a n -> p (a n)")
                        Tyf, Tyb, Tysq, Tmu, Tmsq, Trsc = Txs[0], Txs[1], Txs[1], Txs[2], Txs[2], Txs[2]
                        for tile in range(4):
                            n0 = tile * 512
                            for j in range(4):
                                pc, tpc = bank()
                                for t in range(31):
                                    mm(pc[:], Dg[:, j, t, :], uT[:, j, n0 + t:n0 + t + 512], t == 0, t == 30, [TDg, TuT], tpc, t == 30)
                                act(yf[:, j, :], pc[:], AF.Identity, [tpc, Tc], [Tyf], bias=cv[:, 0, j:j + 1])
                                act(ysq[:, j, :], pc[:], AF.Square, [tpc, Tc], [Tysq], bias=cv[:, 0, j:j + 1])
                                act(yb[:, j, :], pc[:], AF.Identity, [tpc, Tc], [Tyb], bias=cv[:, 0, j:j + 1])
                            p1, tp1 = bank()
                            p2, tp2 = bank()
                            for j in range(4):
                                mm(p1[:], ones_b[:], yb[:, j, :], j == 0, j == 3, [Tyb, Tc], tp1, j == 3)
                            for j in range(4):
                                mm(p2[:], ones_b[:], ysq[:, j, :], j == 0, j == 3, [Tysq, Tc], tp2, j == 3)
                            ts_("dve", mu[:], p1[:], 1.0 / 512, None, ALU.mult, None, [tp1], [Tmu])
                            tt_("dve", msq[:], mu[:], mu[:], ALU.mult, [Tmu], [Tmsq])
                            stt_("dve", msq[:], p2[:], 1.0 / 512, msq[:], ALU.mult, ALU.subtract, [tp2, Tmsq], [Tmsq])
                            rsqrt_(rsc[:], msq[:], 1.0, [Tmsq], Trsc)
                            for j in range(4):
                                tt_("dve", yf[:, j, :], yf[:, j, :], mu[:], ALU.subtract, [Tyf, Tmu], [Tyf])
                                tt_("dve", yf[:, j, :], yf[:, j, :], rsc[:], ALU.mult, [Tyf, Trsc], [Tyf])
                                act(cTt[:, j, n0:n0 + 512], yf[:, j, :], AF.Silu, [Tyf, Tc], [TcT], bias=cv[:, 2, j:j + 1], scale=cv[:, 1, j:j + 1])
                        if b == 0:
                            dump("d_cT", cTt[:].rearrange("p a b -> p (a b)"), [128, 4 * SEQ], TcT, BF16)
                        P.flush()
                if stop <= 5:
                    break
                if stop < 6 and b > 0:
                    break
                gen["list"] = [0, 1, 2, 3, 4, 5, 6]
                with ExitStack() as s57:
                    wao = sbt(s57, "wao", [128, 4, D], BF16)
                    wco = sbt(s57, "wco", [128, 4, D], BF16)
                    wo = sbt(s57, "wo", [128, 8, D], BF16)
                    Twao, Twco, Two = T("wao"), T("wco"), T("wo")
                    ld("pool", wao[:], w_ao[:, :].rearrange("(k p) n -> p k n", p=128), [Twao])
                    ld("pool", wco[:], w_co[:, :].rearrange("(k p) n -> p k n", p=128), [Twco])
                    ld("pool", wo[:], w_o[:, :].rearrange("(k p) n -> p k n", p=128), [Two])
                    wgs = [sbt(s57, "wgs%d" % i, [128, 2, 8, 128], BF16) for i in range(2)]
                    Twgs = [T("wgs0"), T("wgs1")]
                    sga = sbt(s57, "sga", [128, 512], BF16)
                    sgb = sbt(s57, "sgb", [128, 512], BF16)
                    t1 = sbt(s57, "t1", [128, 512], F32)
                    t2 = sbt(s57, "t2", [128, 512], F32)
                    Tsga, Tsgb, Tt1, Tt2 = T("sga"), T("sgb"), T("t1"), T("t2")
                    mg = sbt(s57, "mg", [128, 8, 512], BF16)
                    Tmg = T("mg")
                    mg2 = sbt(s57, "mg2", [128, 8, 512], BF16)
                    Tmg2 = T("mg2")
                    xc = [sbt(s57, "xc%d" % i, [128, 512], F32) for i in range(3)]
                    Txc = [T("xc%d" % i) for i in range(3)]
                    xm = sbt(s57, "xm", [128, 8, 512], F32)
                    Txm = T("xm")
                    sq2 = [sbt(s57, "sq2%d" % i, [128, 512], BF16) for i in range(2)]
                    Tsq2 = [T("sq20"), T("sq21")]
                    rstd2 = sbt(s57, "rstd2", [128, 512], F32)
                    Trstd2 = T("rstd2")
                    h2f = [sbt(s57, "h2f%d" % i, [128, 512], F32) for i in range(2)]
                    Th2f = [T("h2f0"), T("h2f1")]
                    h2b = sbt(s57, "h2b", [128, 8, 512], BF16)
                    h2l = sbt(s57, "h2l", [128, 8, 512], BF16)
                    Th2l = T("h2l")
                    Th2b = T("h2b")
                    h2tok = [sbt(s57, "h2tok%d" % i, [128, D], BF16) for i in range(2)]
                    Th2tok = [T("h2tok0"), T("h2tok1")]
                    xmtok = [sbt(s57, "xmtok%d" % i, [128, D], F32) for i in range(2)]
                    Txmtok = [T("xmtok0"), T("xmtok1")]
                    L = sbt(s57, "L", [128, 4, 36], F32)
                    gmax = sbt(s57, "gmax", [128, 4], F32)
                    gsh = sbt(s57, "gsh", [128, 4, 4], F32)
                    G1h = sbt(s57, "G1h", [128, 4, 4], F32)
                    ge = sbt(s57, "ge", [128, 4, 4], F32)
                    gsum = sbt(s57, "gsum", [128, 4], F32)
                    pgrp = sbt(s57, "pgrp", [128, 4], F32)
                    mskb = sbt(s57, "mskb", [128, 4, 4], F32)
                    elm = sbt(s57, "elm", [128, 4, 32], F32)
                    elm2 = sbt(s57, "elm2", [128, 4, 32], F32)
                    m1 = sbt(s57, "m1", [128, 4], F32)
                    m2 = sbt(s57, "m2", [128, 4], F32)
                    OH1 = sbt(s57, "OH1", [128, 4, 32], F32)
                    OH2 = sbt(s57, "OH2", [128, 4, 32], F32)
                    Ind = sbt(s57, "Ind", [128, 4, 32], F32)
                    Indb = sbt(s57, "Indb", [128, 4, 32], BF16)
                    Accb = sbt(s57, "Accb", [128, 32], BF16)
                    dd = sbt(s57, "dd", [128, 4], F32)
                    ed = sbt(s57, "ed", [128, 4], F32)
                    w1 = sbt(s57, "w1", [128, 4], F32)
                    w2 = sbt(s57, "w2", [128, 4], F32)
                    tmpR = sbt(s57, "tmpR", [128, 4, 32], F32)
                    prod = sbt(s57, "prod", [128, 4, 32], F32)
                    d1f = sbt(s57, "d1f", [128, 4], F32)
                    d2f = sbt(s57, "d2f", [128, 4], F32)
                    accd = sbt(s57, "accd", [128, 32], F32)
                    TR = T("route")
                    mgs = [mg, mg2]
                    Tmgs = [Tmg, Tmg2]
                    plgs = {}

                    def merge_step(tile, m):
                        n0 = tile * 512
                        tsl = slice(n0, n0 + 512)
                        mg_, Tmg_ = mgs[tile % 2], Tmgs[tile % 2]
                        wg_, twg = wgs[m % 2], Twgs[m % 2]
                        ld("sp", wg_[:].rearrange("p a k n -> p (a k n)"), wgs_d[m], [twg], reads=[Twgd])
                        pya, tya = bank()
                        pyc, tyc = bank()
                        pga, tga = bank()
                        pgb, tgb = bank()
                        for k in range(4):
                            mm(pya[:], wao[:, k, m * 128:(m + 1) * 128], attT[:, k, tsl], k == 0, k == 3, [Twao, TattT], tya, k == 3)
                        for k in range(4):
                            mm(pyc[:], wco[:, k, m * 128:(m + 1) * 128], cTt[:, k, tsl], k == 0, k == 3, [Twco, TcT], tyc, k == 3)
                        for k in range(8):
                            mm(pga[:], wg_[:, 0, k, :], hT[:, k, tsl], k == 0, k == 7, [twg, ThT], tga, k == 7)
                        for k in range(8):
                            mm(pgb[:], wg_[:, 1, k, :], hT[:, k, tsl], k == 0, k == 7, [twg, ThT], tgb, k == 7)
                        act(sga[:], pga[:], AF.Sigmoid, [tga], [Tsga])
                        act(sgb[:], pgb[:], AF.Sigmoid, [tgb], [Tsgb])
                        tt_("dve", t1[:], pya[:], sga[:], ALU.mult, [tya, Tsga], [Tt1])
                        tt_("dve", t2[:], pyc[:], sgb[:], ALU.mult, [tyc, Tsgb], [Tt2])
                        tt_("dve", mg_[:, m, :], t1[:], t2[:], ALU.add, [Tt1, Tt2], [Tmg_])

                    def wo_step(tile, m):
                        n0 = tile * 512
                        mg_, Tmg_ = mgs[tile % 2], Tmgs[tile % 2]
                        xc_, txc = xc[m % 3], Txc[m % 3]
                        ld("sp", xc_[:], xT[b, m, :, n0:n0 + 512], [txc])
                        po, tpo = bank()
                        for k in range(8):
                            mm(po[:], wo[:, k, m * 128:(m + 1) * 128], mg_[:, k, :], k == 0, k == 7, [Two, Tmg_], tpo, k == 7)
                        stt_("dve", xm[:, m, :], po[:], mod[:, 16 + m, b:b + 1], xc_[:], ALU.mult, ALU.add, [tpo, Tmod, txc], [Txm])

                    def norm2_step(tile):
                        pss, tpss = bank()
                        for m in range(8):
                            act(sq2[m % 2][:], xm[:, m, :], AF.Square, [Txm], [Tsq2[m % 2]])
                            mm(pss[:], ones_b[:], sq2[m % 2][:], m == 0, m == 7, [Tsq2[m % 2], Tc], tpss, True)
                        rsqrt_(rstd2[:], pss[:], 1.0 / D, [tpss], Trstd2)


                    def h2_step(tile, k):
                        hf, thf = h2f[k % 2], Th2f[k % 2]
                        tt_("dve", hf[:], xm[:, k, :], rstd2[:], ALU.mult, [Txm, Trstd2], [thf])
                        act(hf[:], hf[:], AF.Identity, [thf, Tmod], [thf], bias=mod[:, 24 + k, b:b + 1], scale=A2[:, k, b:b + 1])
                        cp("act", h2b[:, k, :], hf[:], [thf], [Th2b])
                        tt_("dve", h2l[:, k, :], hf[:], h2b[:, k, :], ALU.subtract, [thf, Th2b], [Th2l])

                    def router_step(tile):
                        pl_, tpl_ = bank()
                        plgs[tile] = (pl_, tpl_)
                        for sub in range(4):
                            ssl = slice(sub * 128, (sub + 1) * 128)
                            o_ = pl_[:, sub * 36:(sub + 1) * 36]
                            for k in range(8):
                                mm(o_, h2b[:, k, ssl], wrh[:, k, :], k == 0, False, [Th2b, Tc], tpl_, False)
                                mm(o_, h2b[:, k, ssl], wrl[:, k, :], False, False, [Th2b, Tc], tpl_, False)
                                mm(o_, h2l[:, k, ssl], wrh[:, k, :], False, k == 7, [Th2l, Tc], tpl_, k == 7)

                    def routing_step(tile):
                        gt = b * 4 + tile
                        pl_, tpl_ = plgs[tile]
                        tt_("dve", L[:], pl_[:, 0:144].rearrange("p (s e) -> p s e", s=4), br[:], ALU.add, [tpl_, Tc], [TR])
                        gl = L[:, :, 0:4]
                        red("dve", gmax[:], gl, ALU.max, [TR], [TR])
                        tt_("dve", gsh[:], gl, gmax[:].unsqueeze(2).to_broadcast([128, 4, 4]), ALU.subtract, [TR], [TR])
                        ts_("dve", G1h[:], gsh[:], 0.0, None, ALU.is_equal, None, [TR], [TR])
                        act(ge[:], gsh[:], AF.Exp, [TR], [TR])
                        red("dve", gsum[:], ge[:], ALU.add, [TR], [TR])
                        P.op("dve", lambda e: e.reciprocal(out=pgrp[:], in_=gsum[:]), reads=[TR], writes=[TR])
                        ts_("dve", mskb[:], G1h[:], BIG, -BIG, ALU.mult, ALU.add, [TR], [TR])
                        el4 = L[:, :, 4:36].rearrange("p s (g j) -> p s g j", g=4)
                        elm4 = elm[:].rearrange("p s (g j) -> p s g j", g=4)
                        tt_("dve", elm4, el4, G1h[:].unsqueeze(3).to_broadcast([128, 4, 4, 8]), ALU.mult, [TR], [TR])
                        tt_("dve", elm4, elm4, mskb[:].unsqueeze(3).to_broadcast([128, 4, 4, 8]), ALU.add, [TR], [TR])
                        red("dve", m1[:], elm[:], ALU.max, [TR], [TR])
                        tt_("dve", OH1[:], elm[:], m1[:].unsqueeze(2).to_broadcast([128, 4, 32]), ALU.is_equal, [TR], [TR])
                        stt_("dve", elm2[:], OH1[:], -BIG, elm[:], ALU.mult, ALU.add, [TR], [TR])
                        red("dve", m2[:], elm2[:], ALU.max, [TR], [TR])
                        tt_("dve", OH2[:], elm2[:], m2[:].unsqueeze(2).to_broadcast([128, 4, 32]), ALU.is_equal, [TR], [TR])
                        tt_("dve", dd[:], m2[:], m1[:], ALU.subtract, [TR], [TR])
                        act(ed[:], dd[:], AF.Exp, [TR], [TR])
                        ts_("dve", w1[:], ed[:], 1.0, None, ALU.add, None, [TR], [TR])
                        P.op("dve", lambda e: e.reciprocal(out=w1[:], in_=w1[:]), reads=[TR], writes=[TR])
                        tt_("dve", w2[:], ed[:], w1[:], ALU.mult, [TR], [TR])
                        tt_("dve", Wk[:, gt * 4:(gt + 1) * 4, 0], w1[:], pgrp[:], ALU.mult, [TR], [TWk])
                        tt_("dve", Wk[:, gt * 4:(gt + 1) * 4, 1], w2[:], pgrp[:], ALU.mult, [TR], [TWk])
                        tt_("dve", Ind[:], OH1[:], OH2[:], ALU.add, [TR], [TR])
                        cp("dve", Indb[:], Ind[:], [TR], [TR])
                        cp("dve", Accb[:], Acc[:], [TAcc], [TR])
                        pr, tpr = bank()
                        for sub in range(4):
                            ops_ = [(ones_b, Accb[:])] + [(ones_b, Indb[:, s_, :]) for s_ in range(sub)] + [(utri_b, Indb[:, sub, :])]
                            for i_, (l_, r_) in enumerate(ops_):
                                mm(pr[:, sub * 32:(sub + 1) * 32], l_[:], r_, i_ == 0, i_ == len(ops_) - 1, [TR, TAcc, Tc], tpr, i_ == len(ops_) - 1)
                        tt_("dve", tmpR[:], pr[:, 0:128].rearrange("p (s e) -> p s e", s=4), ecap[:], ALU.add, [tpr, Tc], [TR])
                        tt_("dve", prod[:], tmpR[:], OH1[:], ALU.mult, [TR], [TR])
                        red("dve", d1f[:], prod[:], ALU.add, [TR], [TR])
                        tt_("dve", prod[:], tmpR[:], OH2[:], ALU.mult, [TR], [TR])
                        red("dve", d2f[:], prod[:], ALU.add, [TR], [TR])
                        cp("dve", Dk[:, gt * 4:(gt + 1) * 4, 0], d1f[:], [TR], [TDk])
                        cp("dve", Dk[:, gt * 4:(gt + 1) * 4, 1], d2f[:], [TR], [TDk])
                        red("dve", accd[:], Ind[:].rearrange("p s e -> p e s"), ALU.add, [TR], [TR])
                        tt_("dve", Acc[:], Acc[:], accd[:], ALU.add, [TR, TAcc], [TAcc])

                    def trans_step(tile, sub):
                        n0 = tile * 512
                        gt = b * 4 + tile
                        ssl = slice(sub * 128, (sub + 1) * 128)
                        for k in range(8):
                            tr(pbf[:, k * 128:(k + 1) * 128], h2b[:, k, ssl], ident_b[:], [Th2b, Tc], Tpbf, k == 7)
                        ht, tht = h2tok[sub % 2], Th2tok[sub % 2]
                        cp("act", ht[:], pbf[:], [Tpbf], [tht])
                        for kk in range(2):
                            off = Dk[:, gt * 4 + sub, kk:kk + 1]
                            P.dma("pool", (lambda o_, h_: (lambda e: e.indirect_dma_start(
                                out=Xs[:, :], out_offset=bass.IndirectOffsetOnAxis(ap=o_, axis=0), in_=h_, in_offset=None)))(off, ht[:]),
                                reads=[tht, TDk], writes=[TXs])
                        pxa, tpxa = bank()
                        pxb, tpxb = bank()
                        for k in range(4):
                            tr(pxa[:, k * 128:(k + 1) * 128], xm[:, k, ssl], ident_f[:], [Txm, Tc], tpxa, k == 3)
                        for k in range(4):
                            tr(pxb[:, k * 128:(k + 1) * 128], xm[:, 4 + k, ssl], ident_f[:], [Txm, Tc], tpxb, k == 3)
                        xt_, txt = xmtok[sub % 2], Txmtok[sub % 2]
                        cp("dve", xt_[:, 0:512], pxa[:], [tpxa], [txt])
                        cp("act", xt_[:, 512:1024], pxb[:], [tpxb], [txt])
                        tok0 = b * SEQ + n0 + sub * 128
                        ld("sp", xmid[tok0:tok0 + 128, :], xt_[:], [Txmid], reads=[txt])


                    def post_steps(tile):
                        st = [(lambda m=m: wo_step(tile, m)) for m in range(8)]
                        st.append(lambda: norm2_step(tile))
                        st += [(lambda k=k: h2_step(tile, k)) for k in range(8)]
                        st.append(lambda: router_step(tile))
                        st.append(lambda: routing_step(tile))
                        st += [(lambda sub=sub: trans_step(tile, sub)) for sub in range(4)]
                        return st

                    for m in range(8):
                        merge_step(0, m)
                    for tile in range(4):
                        ps_ = post_steps(tile)
                        ms_ = [(lambda m=m: merge_step(tile + 1, m)) for m in range(8)] if tile < 3 else []
                        pi_, mi_ = 0, 0
                        while pi_ < len(ps_) or mi_ < len(ms_):
                            for _ in range(3):
                                if pi_ < len(ps_):
                                    ps_[pi_]()
                                    pi_ += 1
                            if mi_ < len(ms_):
                                ms_[mi_]()
                                mi_ += 1
                    P.flush()
                gen["list"] = [0, 1, 2, 3, 4]
                if stop <= 6:
                    break
        if dbg and stop > 5.4:
            dump("d_Dk", Dk[:].rearrange("p a b -> p (a b)"), [128, 128], TDk, I32)
            dump("d_Wk", Wk[:].rearrange("p a b -> p (a b)"), [128, 128], TWk)
            d_ = dout("d_xmid", [SEQ, D])
            ld("sp", d_, xmid[0:SEQ, :], [T()], reads=[Txmid])
        if stop <= 6:
            finish()
            return nc, dbg_out
        P.flush()
        ps_es.close()
        ps2 = ExitStack()
        banks2 = [ps2.enter_context(nc.psum_tensor("qb%d" % i, [128, 512], F32)) for i in range(6)]
        bankT2 = [T("qb%d" % i, excl=True) for i in range(6)]
        pbf2 = [ps2.enter_context(nc.psum_tensor("qbf%d" % i, [128, 1024], BF16)) for i in range(2)]
        Tpbf2 = [T("qbf0", excl=True), T("qbf1", excl=True)]
        gen["banks"], gen["bankT"], gen["list"], gen["i"] = banks2, bankT2, [0, 1, 2, 3, 4, 5], 0
        with ExitStack() as s2:
            wge = [sbt(s2, "wge%d" % i, [128, 8, 512], BF16) for i in range(2)]
            wue = [sbt(s2, "wue%d" % i, [128, 8, 512], BF16) for i in range(2)]
            wde = [sbt(s2, "wde%d" % i, [128, 4, D], BF16) for i in range(2)]
            Twe = [T("we0"), T("we1")]
            xg = [sbt(s2, "xg%d" % i, [128, 4, D], BF16) for i in range(3)]
            Txg = [T("xg%d" % i) for i in range(3)]
            XT = [sbt(s2, "XT%d" % i, [128, 8, 512], BF16) for i in range(2)]
            TXT = [T("XT0"), T("XT1")]
            sgf = [sbt(s2, "sgf%d" % i, [128, 512], F32) for i in range(2)]
            Tsgf = [T("sgf0"), T("sgf1")]
            AT = [sbt(s2, "AT%d" % i, [128, 4, 512], BF16) for i in range(2)]
            TAT = [T("AT0"), T("AT1")]
            yo = [sbt(s2, "yo%d" % i, [128, D], F32) for i in range(4)]
            Tyo = [T("yo%d" % i) for i in range(4)]
            ngrp = cap // 512
            groups = [(e_, g_) for e_ in range(NE) for g_ in range(ngrp)]
            cnt2 = {"tp": 0, "yi": 0}

            def ld_w(e_):
                we_ = e_ % 2
                ld("pool", wge[we_][:], w_g[e_].rearrange("(k p) n -> p k n", p=128), [Twe[we_]])
                ld("pool", wue[we_][:], w_u[e_].rearrange("(k p) n -> p k n", p=128), [Twe[we_]])
                ld("pool", wde[we_][:], w_d[e_].rearrange("(k p) n -> p k n", p=128), [Twe[we_]])

            def ld_x(gi):
                e_, g_ = groups[gi]
                s0 = e_ * cap + g_ * 512
                ld("sp", xg[gi % 3][:], Xs[s0:s0 + 512, :].rearrange("(blk p) d -> p blk d", p=128), [Txg[gi % 3]], reads=[TXs])

            def st_T(gi):
                xg_, txg = xg[gi % 3], Txg[gi % 3]
                xt_, txt = XT[gi % 2], TXT[gi % 2]
                for k2 in range(4):
                    pb_, tpb_ = pbf2[cnt2["tp"] % 2], Tpbf2[cnt2["tp"] % 2]
                    cnt2["tp"] += 1
                    for kk in range(2):
                        k = k2 * 2 + kk
                        for blk in range(4):
                            tr(pb_[:, kk * 512 + blk * 128:kk * 512 + (blk + 1) * 128], xg_[:, blk, k * 128:(k + 1) * 128], ident_b[:],
                               [txg, Tc], tpb_, kk == 1 and blk == 3)
                    cp("act" if k2 % 2 == 0 else "dve", xt_[:, k2 * 2:k2 * 2 + 2, :].rearrange("p a b -> p (a b)"), pb_[:], [tpb_], [txt])

            def st_GU(gi):
                e_, g_ = groups[gi]
                we_ = e_ % 2
                xt_, txt = XT[gi % 2], TXT[gi % 2]
                at_, tat = AT[gi % 2], TAT[gi % 2]
                for j in range(4):
                    pg, tpg = bank()
                    pu, tpu = bank()
                    for k in range(8):
                        mm(pg[:], wge[we_][:, k, j * 128:(j + 1) * 128], xt_[:, k, :], k == 0, k == 7, [Twe[we_], txt], tpg, k == 7)
                    for k in range(8):
                        mm(pu[:], wue[we_][:, k, j * 128:(j + 1) * 128], xt_[:, k, :], k == 0, k == 7, [Twe[we_], txt], tpu, k == 7)
                    sg_, tsg_ = sgf[j % 2], Tsgf[j % 2]
                    act(sg_[:], pg[:], AF.Silu, [tpg], [tsg_])
                    tt_("dve", at_[:, j, :], pu[:], sg_[:], ALU.mult, [tpu, tsg_], [tat])

            def st_D(gi):
                e_, g_ = groups[gi]
                we_ = e_ % 2
                s0 = e_ * cap + g_ * 512
                at_, tat = AT[gi % 2], TAT[gi % 2]
                for blk in range(4):
                    yo_, tyo = yo[cnt2["yi"] % 4], Tyo[cnt2["yi"] % 4]
                    cnt2["yi"] += 1
                    for half in range(2):
                        py, tpy = bank()
                        for j in range(4):
                            mm(py[:], at_[:, j, blk * 128:(blk + 1) * 128], wde[we_][:, j, half * 512:(half + 1) * 512], j == 0, j == 3,
                               [tat, Twe[we_]], tpy, j == 3)
                        cp("act" if half == 0 else "dve", yo_[:, half * 512:(half + 1) * 512], py[:], [tpy], [tyo])
                    ld("sp", Ys[s0 + blk * 128:s0 + (blk + 1) * 128, :], yo_[:], [TYs], reads=[tyo])

            cnt_i = sbt(s2, "cnt_i", [128, NE], I32)
            Accb2 = sbt(s2, "Accb2", [128, NE], BF16)
            Tcnt = T("cnt")
            cp("dve", Accb2[:], Acc[:], [TAcc], [Tcnt])
            pbc, tbc = bank()
            mm(pbc[:, 0:NE], ones_b[:], Accb2[:], True, True, [Tcnt, Tc], tbc, True)
            cp("dve", cnt_i[:], pbc[:, 0:NE], [tbc], [Tcnt])
            P.cnt_ap = cnt_d
            ev_ = P.dma("sp", lambda e: e.dma_start(out=cnt_d[0:1, :], in_=cnt_i[0:1, :]), reads=[Tcnt], writes=[T()])
            P.cnt_event = (ev_[0], ev_[1])

            def cond_of(gi, what="x"):
                e_, g_ = groups[gi]
                return None if g_ == 0 else (e_, g_ * 512)

            ng = len(groups)
            ld_w(0)
            ld_x(0)
            P.set_cond(cond_of(1, "x"))
            ld_x(1)
            P.set_cond(None)
            st_T(0)
            for gi in range(ng):
                e_, g_ = groups[gi]
                if g_ == 0 and e_ + 1 < NE:
                    P.set_cond(None)
                    ld_w(e_ + 1)
                if gi + 2 < ng:
                    P.set_cond(cond_of(gi + 2, "x"))
                    ld_x(gi + 2)
                P.set_cond(cond_of(gi, "g"))
                st_GU(gi)
                if gi + 1 < ng:
                    P.set_cond(cond_of(gi + 1, "t"))
                    st_T(gi + 1)
                P.set_cond(cond_of(gi, "d"))
                st_D(gi)
            P.set_cond(None)
            P.flush()
        if stop <= 7:
            finish()
            return nc, dbg_out
        with ExitStack() as s3:
            fn = sbt(s3, "fn", [128, D], F32)
            G2 = sbt(s3, "G2", [128, D], F32)
            dgh = sbt(s3, "dgh", [128, 128], BF16)
            dgl = sbt(s3, "dgl", [128, 128], BF16)
            dgf = sbt(s3, "dgf", [128, 128], F32)
            dgr = sbt(s3, "dgr", [128, 128], F32)
            Tfn, TG2, Tdg = T("fn"), T("G2"), T("dg")
            NB3 = 4
            Y1 = [sbt(s3, "Y1%d" % i, [128, D], F32) for i in range(NB3)]
            Y2 = [sbt(s3, "Y2%d" % i, [128, D], F32) for i in range(NB3)]
            X3 = [sbt(s3, "X3%d" % i, [128, D], F32) for i in range(NB3)]
            F3 = [sbt(s3, "F3%d" % i, [128, D], F32) for i in range(NB3)]
            J3 = sbt(s3, "J3", [128, D], F32)
            ssq = [sbt(s3, "ssq%d" % i, [128, 1], F32) for i in range(NB3)]
            TY1, TY2, TX3, TF3, Tss = [[T() for _ in range(NB3)] for _ in range(5)]
            TJ3 = T("J3")
            ld("sp", fn[:], fnorm[:, :], [Tfn])
            tiles3 = [(b, tl) for b in range(NB) for tl in range(16)]

            def p1(it):
                b, tl = tiles3[it]
                i2 = it % NB3
                if tl == 0:
                    for k in range(8):
                        ts_("dve", dgf[:], ident_f[:], mod[:, 40 + k, b:b + 1], None, ALU.mult, None, [Tc, Tmod, Tdg], [Tdg])
                        cp("dve", dgh[:], dgf[:], [Tdg], [Tdg])
                        tt_("dve", dgr[:], dgf[:], dgh[:], ALU.subtract, [Tdg], [Tdg])
                        cp("dve", dgl[:], dgr[:], [Tdg], [Tdg])
                        pb, tb = bank()
                        mm(pb[:, 0:128], ones_b[:], dgh[:], True, False, [Tc, Tdg], tb, False)
                        mm(pb[:, 0:128], ones_b[:], dgl[:], False, True, [Tc, Tdg], tb, True)
                        cp("act", G2[:, k * 128:(k + 1) * 128], pb[:, 0:128], [tb] + TF3, [TG2])

                idx = (b * 4 + tl // 4) * 4 + tl % 4
                tok0 = b * SEQ + tl * 128
                for kk, (Y_, TY_) in enumerate(((Y1[i2], TY1[i2]), (Y2[i2], TY2[i2]))):
                    off = Dk[:, idx, kk:kk + 1]
                    P.dma("pool", (lambda o_, y_: (lambda e: e.indirect_dma_start(
                        out=y_, out_offset=None, in_=Ys[:, :], in_offset=bass.IndirectOffsetOnAxis(ap=o_, axis=0))))(off, Y_[:]),
                        reads=[TYs, TDk], writes=[TY_])
                ld("sp", X3[i2][:], xmid[tok0:tok0 + 128, :], [TX3[i2]], reads=[Txmid])
                act(F3[i2][:], Y1[i2][:], AF.Copy, [TY1[i2], TWk], [TF3[i2]], scale=Wk[:, idx, 0:1])
                stt_("dve", F3[i2][:], Y2[i2][:], Wk[:, idx, 1:2], F3[i2][:], ALU.mult, ALU.add, [TY2[i2], TWk, TF3[i2]], [TF3[i2]])
                tt_("dve", F3[i2][:], F3[i2][:], G2[:], ALU.mult, [TF3[i2], TG2], [TF3[i2]])
                tt_("dve", X3[i2][:], X3[i2][:], F3[i2][:], ALU.add, [TX3[i2], TF3[i2]], [TX3[i2]])
                mset("dve", ssq[i2][:], 0.0, [Tss[i2]])
                act(J3[:], X3[i2][:], AF.Square, [TX3[i2], Tss[i2]], [TJ3, Tss[i2]], accum=ssq[i2][:])
                act(ssq[i2][:], ssq[i2][:], AF.Sqrt, [Tss[i2], Tc], [Tss[i2]], bias=epsc[:, 0:1], scale=1.0 / D)

            def p2(it):
                b, tl = tiles3[it]
                i2 = it % NB3
                tok0 = b * SEQ + tl * 128
                P.op("dve", lambda e: e.reciprocal(out=ssq[i2][:], in_=ssq[i2][:]), reads=[Tss[i2]], writes=[Tss[i2]])
                stt_("dve", X3[i2][:], X3[i2][:], ssq[i2][:, 0:1], fn[:], ALU.mult, ALU.mult, [TX3[i2], Tss[i2], Tfn], [TX3[i2]])
                ld("sp", out[tok0:tok0 + 128, :], X3[i2][:], [Tout], reads=[TX3[i2]])

            n3 = len(tiles3)
            p1(0)
            for it in range(n3):
                if it + 1 < n3:
                    p1(it + 1)
                p2(it)
            P.flush()
        finish()
        return nc, dbg_out


def host_prep(inputs, core):
    f = np.float32
    bs = slice(NB * core, NB * core + NB)
    x = np.asarray(inputs["x"][bs], f)
    ctx = np.asarray(inputs["ctx"][bs], f)
    m = {}
    m["xT"] = np.ascontiguousarray(x.transpose(0, 2, 1)).reshape(NB, 8, 128, SEQ)
    m["ctxT"] = np.ascontiguousarray(ctx.transpose(0, 2, 1)).reshape(NB, 8, 128, NCTX)
    cc = np.concatenate([np.asarray(inputs["c"][bs], f), np.asarray(inputs["c_ctx"], f)[None]], 0)
    m["cT"] = np.ascontiguousarray(cc.reshape(5, 8, 128).transpose(2, 1, 0)).reshape(128, 40)
    return m


def host_shared(inputs):
    f = np.float32
    g = lambda k: np.asarray(inputs[k], f)[0]
    s = {}
    s["w_mod"] = np.ascontiguousarray(g("w_mod"))
    s["b_modT"] = np.ascontiguousarray(g("b_mod").reshape(48, 128).T)
    s["nmixT"] = np.ascontiguousarray(g("norm_mix").reshape(8, 128).T)
    s["nffnT"] = np.ascontiguousarray(g("norm_ffn").reshape(8, 128).T)
    s["w_in"] = np.ascontiguousarray(g("w_in"))
    rpb = g("rpb")
    p = np.arange(128)
    half, ck = p // 64, p % 64
    cq = np.arange(64)
    c_start = np.clip(cq - 8, 0, 48)
    mask = (ck[:, None] >= c_start[None, :]) & (ck[:, None] < c_start[None, :] + 16)
    dc = np.clip(ck[:, None] - cq[None, :] + 15, 0, 30)
    tt = np.empty((128, 4, 15, 64), f)
    for hp in range(4):
        h = 2 * hp + half
        for jj in range(15):
            v = rpb[h[:, None], 14 - jj, dc]
            tt[:, hp, jj, :] = np.where(mask, v, f(NEG))
    s["tt"] = tt.reshape(128, -1)
    s["w_att_out"] = np.ascontiguousarray(g("w_att_out"))
    s["w_conv_out"] = np.ascontiguousarray(g("w_conv_out"))
    s["w_o"] = np.ascontiguousarray(g("w_o"))
    s["cwT"] = np.ascontiguousarray(g("conv_w").reshape(31, 4, 128).transpose(2, 1, 0)).reshape(128, -1)
    cvec = np.stack([g("conv_b"), g("conv_ln_g"), g("conv_ln_b")], 0)
    s["cvec"] = np.ascontiguousarray(cvec.reshape(3, 4, 128).transpose(2, 0, 1)).reshape(128, 12)
    wrc = np.concatenate([g("w_router_group"), g("w_router_expert")], 1)
    s["wr"] = np.ascontiguousarray(wrc.reshape(8, 128, 36).transpose(1, 0, 2)).reshape(128, -1)
    brc = np.concatenate([g("b_router_group"), g("b_router_expert")], 0)
    s["br"] = np.ascontiguousarray(np.broadcast_to(brc[None, None, :], (128, 4, 36))).reshape(128, -1)
    s["ecap"] = np.ascontiguousarray(np.broadcast_to((np.arange(32, dtype=f) * CAP)[None, None, :], (128, 4, 32))).reshape(128, -1)
    s["w_exp_gate"] = np.ascontiguousarray(g("w_exp_gate"))
    s["w_exp_up"] = np.ascontiguousarray(g("w_exp_up"))
    s["w_exp_down"] = np.ascontiguousarray(g("w_exp_down"))
    s["fnorm"] = np.ascontiguousarray(np.broadcast_to(np.asarray(inputs["final_norm"], f)[None, :], (128, D)))
    return s


def kernel(**inputs):
    nc, _ = build()
    shared = host_shared(inputs)
    in_maps = []
    for core in range(8):
        m = dict(shared)
        m.update(host_prep(inputs, core))
        in_maps.append(m)
    res = run_bass_kernel_spmd(nc, in_maps, core_ids=list(range(8)))
    outs = [np.asarray(r["out"]).reshape(NB, SEQ, D) for r in res.results]
    return np.concatenate(outs, 0).astype(np.float32)
```

```python
from contextlib import ExitStack
import numpy as np
import concourse.bass as bass
import concourse.mybir as mybir
from concourse.bass_utils import run_bass_kernel_spmd

F32 = mybir.dt.float32
BF16 = mybir.dt.bfloat16
I32 = mybir.dt.int32
AF = mybir.ActivationFunctionType
ALU = mybir.AluOpType
AX = mybir.AxisListType

D = 1024
SEQ = 2048
NB = 4
NCTX = 256
DIN = 4608
Q0, K0, V0, GLU0, GA0, GB0 = 0, 512, 1024, 1536, 2560, 3584
NE = 32
CAP = 1536
NSLOT = NE * CAP
NTOK = NB * SEQ
EPS = 1e-6
NEG = -30000.0
BIG = 1.0e9

SEM_LIMIT = 12000
ENGS = ("pe", "act", "dve", "pool", "sp")


class T:
    __slots__ = ("name", "w", "r", "excl")

    def __init__(self, name="t", excl=False):
        self.name = name
        self.w = []
        self.r = []
        self.excl = excl


class Prog:
    def __init__(self, nc, es, dma_ring=6):
        self.nc = nc
        self.es = es
        self.q = {e: [] for e in ENGS}
        self.sem = {}
        self.cnt = {}
        self.known = {e: {} for e in ENGS}
        self.nsem = 0
        for e in ENGS:
            self.sem[e] = self._newsem(e)
            self.cnt[e] = 0
        self.ring = {e: [[self._newsem("d" + e), 0] for _ in range(dma_ring)] for e in ("sp", "act", "pool")}
        self.ring_i = {e: 0 for e in ("sp", "act", "pool")}
        self.cbase = None
        self.cond = None
        self.cknown = {e: {} for e in ENGS}
        self.cnt_ap = None
        self.cnt_event = None
        self.regs = {}
        self.loaded = {}

    def set_cond(self, cond):
        if cond != self.cond:
            self.cond = cond
            self.cknown = {e: {} for e in ENGS}
            self.cbase = {id(self.sem[e]): self.cnt[e] for e in ENGS}
            for ring in self.ring.values():
                for slot in ring:
                    self.cbase[id(slot[0])] = slot[1]

    def _newsem(self, tag):
        self.nsem += 1
        return self.es.enter_context(self.nc.semaphore("s%s_%d" % (tag, self.nsem)))

    def _need(self, eng, ev, need, war=False):
        sem, val, src = ev
        if src == eng and eng == "pe":
            return
        if self.known[eng].get(id(sem), -1) >= val:
            return
        if self.cond is not None and self.cknown[eng].get(id(sem), -1) >= val:
            return
        if src is not None and sem is self.sem[src] and val > self.cnt[src]:
            raise RuntimeError("wait on an event whose incrementing instruction is not recorded yet (deadlock risk): %s -> %s" % (src, eng))
        cur = need.get(id(sem))
        if cur is None or cur[1] < val:
            need[id(sem)] = (sem, val)

    def _deps(self, eng, reads, writes, is_dma=False):
        need = {}
        for t in reads:
            for ev in t.w:
                self._need(eng, ev, need)
            if t.excl:
                for ev in t.r:
                    self._need(eng, ev, need, war=True)
        for t in writes:
            for ev in t.w:
                if is_dma and ev[2] is None and not t.r:
                    continue
                self._need(eng, ev, need)
            for ev in t.r:
                self._need(eng, ev, need, war=True)
        waits = []
        for k, (sem, val) in need.items():
            self._learn(eng, k, val)
            waits.append((sem, val))
        return waits

    def _learn(self, eng, k, val):
        if self.cond is None:
            self.known[eng][k] = val
        else:
            self.cknown[eng][k] = val

    def _mark(self, ev, reads, writes):
        for t in reads:
            if ev[2] is not None:
                t.r = [x for x in t.r if x[2] != ev[2]]
            t.r.append(ev)
        for t in writes:
            if ev[2] is None and not t.r:
                t.w = [x for x in t.w if x[2] is None] + [ev]
            else:
                t.w = [ev]
            t.r = []

    def op(self, eng, fn, reads=(), writes=(), inc=True):
        waits = self._deps(eng, reads, writes)
        ev = (self.sem[eng], self.cnt[eng] + 1, eng)
        self.q[eng].append((waits, fn, self.sem[eng] if inc else None, 1, self.cond, self.cbase if self.cond is not None else None))
        self._mark(ev, reads, writes)
        if inc:
            self.cnt[eng] += 1
            if self.cnt[eng] >= SEM_LIMIT:
                self.sem[eng] = self._newsem(eng)
                self.cnt[eng] = 0

    def dma(self, eng, fn, reads=(), writes=()):
        waits = self._deps(eng, reads, writes, is_dma=True)
        ring = self.ring[eng]
        i = self.ring_i[eng]
        self.ring_i[eng] = (i + 1) % len(ring)
        slot = ring[i]
        if slot[1] > 0 and max(self.known[eng].get(id(slot[0]), -1), self.cknown[eng].get(id(slot[0]), -1) if self.cond is not None else -1) < slot[1]:
            self._learn(eng, id(slot[0]), slot[1])
            waits.append((slot[0], slot[1]))
        if slot[1] >= SEM_LIMIT * 4:
            slot[0] = self._newsem("d" + eng)
            slot[1] = 0
        slot[1] += 16
        ev = (slot[0], slot[1], None)
        self.q[eng].append((waits, fn, slot[0], 16, self.cond, self.cbase if self.cond is not None else None))
        self._mark(ev, reads, writes)
        return ev

    def barrier(self):
        evs = []
        for e in ENGS:
            if self.cnt[e] > 0:
                evs.append((self.sem[e], self.cnt[e], e))
        for ring in self.ring.values():
            for slot in ring:
                if slot[1] > 0:
                    evs.append((slot[0], slot[1], None))
        for e in ENGS:
            need = {}
            for ev in evs:
                self._need(e, ev, need)
            waits = []
            for k, (sem, val) in need.items():
                self.known[e][k] = val
                waits.append((sem, val))
            if waits:
                self.q[e].append((waits, None, None, 0, None, 0))

    def flush(self):
        self.barrier()
        self.emit()

    def emit(self):
        q = self.q
        self.q = {e: [] for e in ENGS}
        if not any(q.values()):
            return

        def run(e, seg):
            for waits, fn, sem, n, _c, _v in seg:
                for (s_, v) in waits:
                    e.wait_ge(s_, v)
                if fn is None:
                    continue
                ins = fn(e)
                if sem is not None:
                    ins.then_inc(sem, n)

        def replay(e, lst, name=None):
            i = 0
            while i < len(lst):
                cond = lst[i][4]
                j = i
                while j < len(lst) and lst[j][4] == cond:
                    j += 1
                seg = lst[i:j]
                i = j
                if cond is None:
                    run(e, seg)
                    continue
                ex, thr = cond
                key = (name, ex % 2)
                if key not in self.regs:
                    self.regs[key] = e.alloc_register("cnt_%s_%d" % key)
                reg = self.regs[key]
                if self.loaded.get(key) != ex:
                    if (name, "ev") not in self.loaded:
                        e.wait_ge(self.cnt_event[0], self.cnt_event[1])
                        self.loaded[(name, "ev")] = True
                    e.reg_load(reg, self.cnt_ap[0:1, ex:ex + 1])
                    self.loaded[key] = ex
                tot = {}
                first = seg[0][5]
                for waits, fn, sem, n, _c, va in seg:
                    if fn is not None and sem is not None:
                        tot[id(sem)] = (sem, tot.get(id(sem), (sem, 0))[1] + n)
                if tot:
                    with e.If_lt(reg, thr + 1):
                        for waits, fn, sem, n, _c, va in seg:
                            for (s_, v) in waits:
                                if id(s_) in first and v > first[id(s_)]:
                                    continue
                                e.wait_ge(s_, v)
                        e.drain()
                        for sem, n in tot.values():
                            e.sem_inc(sem, n)
                    with e.Else():
                        run(e, seg)
                else:
                    with e.If_lt(reg, thr + 1):
                        e.nop()
                    with e.Else():
                        run(e, seg)

        with self.nc.Block() as block:
            @block.tensor
            def _(e):
                replay(e, q["pe"], "pe")

            @block.scalar
            def _(e):
                replay(e, q["act"], "act")

            @block.vector
            def _(e):
                replay(e, q["dve"], "dve")

            @block.gpsimd
            def _(e):
                replay(e, q["pool"], "pool")

            @block.sync
            def _(e):
                replay(e, q["sp"], "sp")


def r_start(r):
    return min(max(r - 4, 0), 24)


def build(stop=99, dbg=False, cap=CAP):
    nslot = NE * cap
    nc = bass.Bass("TRN2", target_bir_lowering=False)

    def din(name, shape, dt=F32):
        return nc.dram_tensor(name, list(shape), dt, kind="ExternalInput").ap()

    xT = din("xT", [NB, 8, 128, SEQ])
    ctxT = din("ctxT", [NB, 8, 128, NCTX])
    cT = din("cT", [128, 8 * 5])
    w_mod = din("w_mod", [D, 6 * D])
    b_modT = din("b_modT", [128, 48])
    nmixT = din("nmixT", [128, 8])
    nffnT = din("nffnT", [128, 8])
    w_in = din("w_in", [D, DIN])
    tt_in = din("tt", [128, 4 * 15 * 64])
    w_ao = din("w_att_out", [512, D])
    w_co = din("w_conv_out", [512, D])
    w_o = din("w_o", [D, D])
    cwT = din("cwT", [128, 4 * 31])
    cvec = din("cvec", [128, 12])
    wr_in = din("wr", [128, 8 * 36])
    br_in = din("br", [128, 4 * 36])
    ecap_in = din("ecap", [128, 4 * 32])
    w_g = din("w_exp_gate", [NE, D, 512])
    w_u = din("w_exp_up", [NE, D, 512])
    w_d = din("w_exp_down", [NE, 512, D])
    fnorm = din("fnorm", [128, D])
    out = nc.dram_tensor("out", [NTOK, D], F32, kind="ExternalOutput").ap()
    Xs = nc.dram_tensor("Xs", [nslot + 2 * NTOK, D], BF16).ap()
    Ys = nc.dram_tensor("Ys", [nslot + 2 * NTOK, D], F32).ap()
    xmid = nc.dram_tensor("xmid", [NTOK, D], F32).ap()
    wgs_d = nc.dram_tensor("wgs_d", [8, 128, 2 * 8 * 128], BF16).ap()
    cnt_d = nc.dram_tensor("cnt_d", [1, NE], I32).ap()
    dbg_out = {}

    def dout(name, shape, dt=F32):
        a = nc.dram_tensor(name, list(shape), dt, kind="ExternalOutput").ap()
        dbg_out[name] = a
        return a

    with ExitStack() as es:
        P = Prog(nc, es)

        uid = [0]

        def sbt(scope, name, shape, dt):
            uid[0] += 1
            return scope.enter_context(nc.sbuf_tensor("sb%d_%s" % (uid[0], name), list(shape), dt))

        def mm(o, lhsT, rhs, start, stop_, reads, wt, inc):
            P.op("pe", lambda e: e.matmul(o, lhsT, rhs, start=start, stop=stop_, skip_group_check=True),
                 reads=reads, writes=[wt], inc=inc)

        def tr(o, in_, ident, reads, wt, inc=True):
            P.op("pe", lambda e: e.transpose(o, in_, ident), reads=reads, writes=[wt], inc=inc)

        def act(o, in_, func, reads, writes, bias=None, scale=None, accum=None):
            kw = {}
            if bias is not None:
                kw["bias"] = bias
            if scale is not None:
                kw["scale"] = scale
            if accum is not None:
                kw["accum_out"] = accum
            P.op("act", lambda e: e.activation(out=o, in_=in_, func=func, **kw), reads=reads, writes=writes)

        def tt_(eng, o, a, b, op, reads, writes):
            P.op(eng, lambda e: e.tensor_tensor(out=o, in0=a, in1=b, op=op), reads=reads, writes=writes)

        def ts_(eng, o, a, s1, s2, op0, op1, reads, writes):
            if s2 is None:
                P.op(eng, lambda e: e.tensor_scalar(out=o, in0=a, scalar1=s1, scalar2=None, op0=op0), reads=reads, writes=writes)
            else:
                P.op(eng, lambda e: e.tensor_scalar(out=o, in0=a, scalar1=s1, scalar2=s2, op0=op0, op1=op1), reads=reads, writes=writes)

        def stt_(eng, o, a, s, b, op0, op1, reads, writes):
            P.op(eng, lambda e: e.scalar_tensor_tensor(out=o, in0=a, scalar=s, in1=b, op0=op0, op1=op1), reads=reads, writes=writes)

        def cp(eng, o, a, reads, writes):
            if eng == "act":
                P.op("act", lambda e: e.copy(out=o, in_=a), reads=reads, writes=writes)
            else:
                P.op(eng, lambda e: e.tensor_copy(out=o, in_=a), reads=reads, writes=writes)

        def red(eng, o, a, op, reads, writes):
            P.op(eng, lambda e: e.tensor_reduce(out=o, in_=a, axis=AX.X, op=op), reads=reads, writes=writes)

        def rsqrt_(o, in_, scale, reads, wt):
            act(o, in_, AF.Sqrt, reads + [Tc], [wt], bias=epsc[:, 0:1], scale=scale)
            P.op("dve", lambda e: e.reciprocal(out=o, in_=o), reads=[wt], writes=[wt])

        def mset(eng, o, v, writes):
            P.op(eng, lambda e: e.memset(o, v), writes=writes)

        def ld(q, o, in_, writes, reads=()):
            P.dma(q, lambda e: e.dma_start(out=o, in_=in_), reads=reads, writes=writes)

        def dump(name, src_ap, shape, t, dt=F32):
            if not dbg:
                return
            d = dout(name, shape, dt)
            ld("sp", d, src_ap, writes=[T()], reads=[t])

        NBANK = 7
        ps_es = ExitStack()
        banks = [ps_es.enter_context(nc.psum_tensor("pb%d" % i, [128, 512], F32)) for i in range(NBANK)]
        bankT = [T("pb%d" % i, excl=True) for i in range(NBANK)]
        pbf = ps_es.enter_context(nc.psum_tensor("pbf", [128, 1024], BF16))
        Tpbf = T("pbf", excl=True)
        gen = {"list": [0, 1, 2, 3, 4], "i": 0, "banks": banks, "bankT": bankT}

        def bank():
            i = gen["list"][gen["i"] % len(gen["list"])]
            gen["i"] += 1
            return gen["banks"][i], gen["bankT"][i]

        cs = es
        ident_f = sbt(cs, "ident_f", [128, 128], F32)
        ident_b = sbt(cs, "ident_b", [128, 128], BF16)
        ones_b = sbt(cs, "ones_b", [128, 128], BF16)
        ones_f = sbt(cs, "ones_f", [128, 128], F32)
        shift_b = sbt(cs, "shift_b", [128, 128], BF16)
        onesbd = sbt(cs, "onesbd", [128, 128], BF16)
        onespad = sbt(cs, "onespad", [128, 2, 128], BF16)
        utri_f = sbt(cs, "utri_f", [128, 128], F32)
        iot = sbt(cs, "iot", [128, 128], F32)
        epsc = sbt(cs, "epsc", [128, 1], F32)
        tmpc = sbt(cs, "tmpc", [128, 128], F32)
        TTb = sbt(cs, "TTb", [128, 4, 15 * 64], BF16)
        bmod = sbt(cs, "bmod", [128, 48], F32)
        nmix = sbt(cs, "nmix", [128, 8], F32)
        nffn = sbt(cs, "nffn", [128, 8], F32)
        cw = sbt(cs, "cw", [128, 4, 31], F32)
        cv = sbt(cs, "cv", [128, 3, 4], F32)
        wr = sbt(cs, "wr", [128, 8, 36], F32)
        br = sbt(cs, "br", [128, 4, 36], F32)
        wrh = sbt(cs, "wrh", [128, 8, 36], BF16)
        wrl = sbt(cs, "wrl", [128, 8, 36], BF16)
        utri_b = sbt(cs, "utri_b", [128, 128], BF16)
        ecap = sbt(cs, "ecap", [128, 4, 32], F32)
        mod = sbt(cs, "mod", [128, 48, 5], F32)
        A1 = sbt(cs, "A1", [128, 8, 5], F32)
        A2 = sbt(cs, "A2", [128, 8, 5], F32)
        Acc = sbt(cs, "Acc", [128, 32], F32)
        Wk = sbt(cs, "Wk", [128, 64, 2], F32)
        Dk = sbt(cs, "Dk", [128, 64, 2], I32)
        Tc = T("consts")
        Tmod = T("mod")
        TAcc = T("Acc")
        TWk = T("Wk")
        TDk = T("Dk")
        TXs, TYs, Txmid = T("Xs"), T("Ys"), T("xmid")
        Tout = T("out")
        Twgd = T("wgs_d")

        P.op("pool", lambda e: e.iota(iot[:], pattern=[[1, 128]], base=0, channel_multiplier=-1,
                                      allow_small_or_imprecise_dtypes=True), writes=[Tc])
        ts_("dve", ident_f[:], iot[:], 0.0, None, ALU.is_equal, None, [Tc], [Tc])
        cp("dve", ident_b[:], ident_f[:], [Tc], [Tc])
        ts_("dve", utri_f[:], iot[:], 0.0, None, ALU.is_gt, None, [Tc], [Tc])
        ts_("dve", tmpc[:], iot[:], 64.0, None, ALU.is_equal, None, [Tc], [Tc])
        ts_("dve", ones_f[:], iot[:], -64.0, None, ALU.is_equal, None, [Tc], [Tc])
        tt_("dve", shift_b[:], tmpc[:], ones_f[:], ALU.add, [Tc], [Tc])
        mset("dve", ones_f[:], 1.0, [Tc])
        mset("dve", ones_b[:], 1.0, [Tc])
        mset("dve", onesbd[:], 0.0, [Tc])
        mset("dve", onesbd[0:64, 0:64], 1.0, [Tc])
        mset("dve", onesbd[64:128, 64:128], 1.0, [Tc])
        mset("dve", onespad[:], 0.0, [Tc])
        mset("dve", onespad[:, 0, 0:64], 1.0, [Tc])
        mset("dve", onespad[:, 1, 64:128], 1.0, [Tc])
        mset("dve", Acc[:], 0.0, [TAcc])
        mset("dve", epsc[:], EPS, [Tc])
        ld("pool", TTb[:].rearrange("p a b -> p (a b)"), tt_in[:, :], [Tc])
        ld("sp", bmod[:], b_modT[:, :], [Tc])
        ld("sp", nmix[:], nmixT[:, :], [Tc])
        ld("sp", nffn[:], nffnT[:, :], [Tc])
        ld("sp", cw[:].rearrange("p a b -> p (a b)"), cwT[:, :], [Tc])
        ld("sp", cv[:].rearrange("p a b -> p (a b)"), cvec[:, :], [Tc])
        ld("sp", wr[:].rearrange("p a b -> p (a b)"), wr_in[:, :], [Tc])
        ld("sp", br[:].rearrange("p a b -> p (a b)"), br_in[:, :], [Tc])
        cp("dve", wrh[:], wr[:], [Tc], [Tc])
        tt_("dve", wrl[:], wr[:], wrh[:], ALU.subtract, [Tc], [Tc])
        cp("dve", utri_b[:], utri_f[:], [Tc], [Tc])
        ld("sp", ecap[:].rearrange("p a b -> p (a b)"), ecap_in[:, :], [Tc])

        with ExitStack() as s0:
            cin = sbt(s0, "cin", [128, 8, 5], F32)
            csb = sbt(s0, "csb", [128, 8, 5], BF16)
            wm = [sbt(s0, "wm%d" % i, [128, 8, 512], BF16) for i in range(2)]
            Twm = [T("wm0"), T("wm1")]
            Tcin = T("cin")
            ld("sp", cin[:].rearrange("p a b -> p (a b)"), cT[:, :], [Tcin])
            act(csb[:], cin[:], AF.Silu, [Tcin], [Tcin])
            for j in range(12):
                wb_, tw = wm[j % 2], Twm[j % 2]
                ld("pool", wb_[:], w_mod[:, j * 512:(j + 1) * 512].rearrange("(k p) n -> p k n", p=128), [tw])
                for m in range(4):
                    pb, tb = bank()
                    for k in range(8):
                        mm(pb[:, 0:5], wb_[:, k, m * 128:(m + 1) * 128], csb[:, k, :], k == 0, k == 7, [tw, Tcin], tb, k == 7)
                    ci = j * 4 + m
                    ts_("dve", mod[:, ci, :], pb[:, 0:5], bmod[:, ci:ci + 1], None, ALU.add, None, [tb, Tc], [Tmod])
            ts_("dve", A1[:], mod[:, 8:16, :], 1.0, None, ALU.add, None, [Tmod], [Tmod])
            ts_("dve", A2[:], mod[:, 32:40, :], 1.0, None, ALU.add, None, [Tmod], [Tmod])
            for k in range(8):
                ts_("dve", A1[:, k, :], A1[:, k, :], nmix[:, k:k + 1], None, ALU.mult, None, [Tmod, Tc], [Tmod])
                ts_("dve", A2[:, k, :], A2[:, k, :], nffn[:, k:k + 1], None, ALU.mult, None, [Tmod, Tc], [Tmod])
            dump("d_mod", mod[:].rearrange("p a b -> p (a b)"), [128, 240], Tmod)
            wtmp = [sbt(s0, "wtmp%d" % i, [128, 2, 8, 128], BF16) for i in range(2)]
            Twtmp = [T("wtmp0"), T("wtmp1")]
            for m in range(8):
                wt_, twt = wtmp[m % 2], Twtmp[m % 2]
                ld("pool", wt_[:, 0, :, :], w_in[:, GA0 + m * 128:GA0 + (m + 1) * 128].rearrange("(k p) n -> p k n", p=128), [twt])
                ld("pool", wt_[:, 1, :, :], w_in[:, GB0 + m * 128:GB0 + (m + 1) * 128].rearrange("(k p) n -> p k n", p=128), [twt])
                ld("sp", wgs_d[m], wt_[:].rearrange("p a k n -> p (a k n)"), [Twgd], reads=[twt])
            P.flush()

        def finish():
            P.barrier()
            P.emit()

        if stop <= 0:
            finish()
            return nc, dbg_out

        for b in range(NB):
            with ExitStack() as sq_:
                hT = sbt(sq_, "hT", [128, 8, SEQ], BF16)
                attT = sbt(sq_, "attT", [128, 4, SEQ], BF16)
                cTt = sbt(sq_, "cTt", [128, 4, SEQ], BF16)
                ThT, TattT, TcT = T("hT"), T("attT"), T("cT")
                with ExitStack() as su:
                    uT = sbt(su, "uT", [128, 4, SEQ + 30], BF16)
                    TuT = T("uT")
                    Dg = sbt(su, "Dg", [128, 4, 31, 128], BF16)
                    TDg = T("Dg")
                    for j in range(4):
                        tt_("dve", Dg[:, j, :, :], ident_f[:].unsqueeze(1).to_broadcast([128, 31, 128]),
                            cw[:, j, :].unsqueeze(2).to_broadcast([128, 31, 128]), ALU.mult, [Tc], [TDg])
                    with ExitStack() as s13:
                        hcT = sbt(s13, "hcT", [128, 8, NCTX], BF16)
                        ThcT = T("hcT")
                        xs = [sbt(s13, "xs%d" % i, [128, 8, 256], F32) for i in range(3)]
                        Txs = [T("xs%d" % i) for i in range(3)]
                        sqb = [sbt(s13, "sqb0", [128, 8, 256], BF16)] * 2
                        Tsq = [T("sq0")] * 2
                        rstd = [sbt(s13, "rstd%d" % i, [128, 256], F32) for i in range(2)]
                        Trstd = [T("rstd0"), T("rstd1")]
                        wbuf = [sbt(s13, "wbuf%d" % i, [128, 2, 8, 128], BF16) for i in range(2)]
                        Twb = [T("wb0"), T("wb1")]
                        wqkv = sbt(s13, "wqkv", [128, 3, 8, 128], BF16)
                        Twqkv = T("wqkv")
                        qT = sbt(s13, "qT", [128, SEQ], BF16)
                        Kbd = sbt(s13, "Kbd", [128, 32, 128], BF16)
                        Vbd = sbt(s13, "Vbd", [128, 32, 128], BF16)
                        kc = sbt(s13, "kc", [128, NCTX], BF16)
                        Vcp = sbt(s13, "Vcp", [128, 2, 2, 128], BF16)
                        Vtok = sbt(s13, "Vtok", [128, 512], BF16)
                        TqT, TKbd, TVbd, Tkc, TVcp, TVtok = T("qT"), T("Kbd"), T("Vbd"), T("kc"), T("Vcp"), T("Vtok")
                        Pt = [sbt(s13, "Pt%d" % i, [128, 512], BF16) for i in range(3)]
                        TPt = [T("Pt0"), T("Pt1"), T("Pt2")]
                        rZ = sbt(s13, "rZ", [128, 512], F32)
                        TrZ = T("rZ")
                        sg = [sbt(s13, "sg%d" % i, [128, 512], BF16) for i in range(2)]
                        Tsg = [T("sg0"), T("sg1")]

                        mset("pool", Kbd[:], 0.0, [TKbd])
                        mset("pool", Vbd[:], 0.0, [TVbd])
                        mset("pool", Vcp[:], 0.0, [TVcp])
                        mset("pool", uT[:, :, 0:15], 0.0, [TuT])
                        mset("pool", uT[:, :, SEQ + 15:SEQ + 30], 0.0, [TuT])

                        NT_ = 256
                        jobs = [(ctxT[b], 0, hcT, ThcT, 4)] + [(xT[b], t_ * NT_, hT, ThT, b) for t_ in range(SEQ // NT_)]
                        nj_ = len(jobs)
                        stb = {}

                        def nm_load(it):
                            src, n0, dst, Tdst, bcol = jobs[it]
                            ld("sp", xs[it % 3][:], src[:, :, n0:n0 + NT_].rearrange("k p n -> p k n"), [Txs[it % 3]])

                        def nm_sq(it):
                            x_, tx = xs[it % 3], Txs[it % 3]
                            tt_("dve", sqb[it % 2][:], x_[:], x_[:], ALU.mult, [tx], [Tsq[it % 2]])
                            pb, tb = bank()
                            stb[it] = (pb, tb)
                            for k in range(8):
                                mm(pb[:, 0:NT_], ones_b[:], sqb[it % 2][:, k, :], k == 0, k == 7, [Tsq[it % 2], Tc], tb, k == 7)

                        def nm_rs(it):
                            pb, tb = stb[it]
                            rsqrt_(rstd[it % 2][:], pb[:, 0:NT_], 1.0 / D, [tb], Trstd[it % 2])

                        def nm_B(it):
                            src, n0, dst, Tdst, bcol = jobs[it]
                            x_, tx = xs[it % 3], Txs[it % 3]
                            tt_("dve", x_[:], x_[:], rstd[it % 2][:].unsqueeze(1).to_broadcast([128, 8, NT_]), ALU.mult, [tx, Trstd[it % 2]], [tx])
                            for k in range(8):
                                act(dst[:, k, n0:n0 + NT_], x_[:, k, :], AF.Identity, [tx, Tmod], [Tdst],
                                    bias=mod[:, k, bcol:bcol + 1], scale=A1[:, k, bcol:bcol + 1])

                        nm_load(0)
                        nm_load(1)
                        nm_sq(0)
                        nm_sq(1)
                        nm_rs(0)
                        for it_ in range(nj_):
                            if it_ + 2 < nj_:
                                nm_load(it_ + 2)
                            if it_ + 1 < nj_:
                                nm_rs(it_ + 1)
                            nm_B(it_)
                            if it_ + 2 < nj_:
                                nm_sq(it_ + 2)
                        if b == 0:
                            dump("d_hT", hT[:].rearrange("p a b -> p (a b)"), [128, 8 * SEQ], ThT, BF16)
                            dump("d_hcT", hcT[:].rearrange("p a b -> p (a b)"), [128, 8 * NCTX], ThcT, BF16)
                        if stop <= 1:
                            P.barrier()
                            break

                        it = 0
                        for j in range(4):
                            wgl, twgl = wbuf[j % 2], Twb[j % 2]
                            ld("pool", wgl[:, 0, :, :], w_in[:, GLU0 + j * 128:GLU0 + (j + 1) * 128].rearrange("(k p) n -> p k n", p=128), [twgl])
                            ld("pool", wgl[:, 1, :, :], w_in[:, GLU0 + 512 + j * 128:GLU0 + 512 + (j + 1) * 128].rearrange("(k p) n -> p k n", p=128), [twgl])
                            for t in range(4):
                                pa, ta = bank()
                                pg, tg = bank()
                                for k in range(8):
                                    mm(pa[:], wgl[:, 0, k, :], hT[:, k, t * 512:(t + 1) * 512], k == 0, k == 7, [twgl, ThT], ta, k == 7)
                                for k in range(8):
                                    mm(pg[:], wgl[:, 1, k, :], hT[:, k, t * 512:(t + 1) * 512], k == 0, k == 7, [twgl, ThT], tg, k == 7)
                                s_, tsg = sg[it % 2], Tsg[it % 2]
                                it += 1
                                act(s_[:], pg[:], AF.Sigmoid, [tg], [tsg])
                                tt_("dve", uT[:, j, 15 + t * 512:15 + (t + 1) * 512], pa[:], s_[:], ALU.mult, [ta, tsg], [TuT])
                        if b == 0:
                            dump("d_uT", uT[:].rearrange("p a b -> p (a b)"), [128, 4 * (SEQ + 30)], TuT, BF16)
                        if stop <= 2:
                            P.barrier()
                            break

                        gen["list"] = [0, 1, 2]
                        for hp in range(4):
                            for i, c0 in enumerate((Q0, K0, V0)):
                                ld("pool", wqkv[:, i, :, :], w_in[:, c0 + hp * 128:c0 + (hp + 1) * 128].rearrange("(k p) n -> p k n", p=128), [Twqkv])
                            pb, tb = bank()
                            for k in range(8):
                                mm(pb[:, 0:NCTX], wqkv[:, 1, k, :], hcT[:, k, :], k == 0, k == 7, [Twqkv, ThcT], tb, k == 7)
                            cp("act", kc[:], pb[:, 0:NCTX], [tb], [Tkc])
                            pb, tb = bank()
                            for cc in range(2):
                                for k in range(8):
                                    mm(pb[:, cc * 128:(cc + 1) * 128], hcT[:, k, cc * 128:(cc + 1) * 128], wqkv[:, 2, k, :], k == 0, k == 7, [Twqkv, ThcT], tb, k == 7)
                            pv3 = pb[:, 0:256].rearrange("p (c f) -> p c f", c=2)
                            cp("dve", Vcp[:, :, 0, 0:64], pv3[:, :, 0:64], [tb], [TVcp])
                            cp("dve", Vcp[:, :, 1, 64:128], pv3[:, :, 64:128], [tb], [TVcp])
                            if stop <= 2.1:
                                continue
                            for t in range(4):
                                pb, tb = bank()
                                for k in range(8):
                                    mm(pb[:], wqkv[:, 0, k, :], hT[:, k, t * 512:(t + 1) * 512], k == 0, k == 7, [Twqkv, ThT], tb, k == 7)
                                act(qT[:, t * 512:(t + 1) * 512], pb[:], AF.Copy, [tb], [TqT], scale=0.125)
                                pb, tb = bank()
                                for k in range(8):
                                    mm(pb[:], wqkv[:, 1, k, :], hT[:, k, t * 512:(t + 1) * 512], k == 0, k == 7, [Twqkv, ThT], tb, k == 7)
                                cp("dve", Kbd[0:64, t * 8:(t + 1) * 8, 0:64], pb[0:64, :].rearrange("p (r c) -> p r c", r=8), [tb], [TKbd])
                                cp("act", Kbd[64:128, t * 8:(t + 1) * 8, 64:128], pb[64:128, :].rearrange("p (r c) -> p r c", r=8), [tb], [TKbd])
                            if stop <= 2.2:
                                continue
                            for g4 in range(4):
                                pv, tv = bank()
                                for tq in range(4):
                                    tt16 = g4 * 4 + tq
                                    for k in range(8):
                                        mm(pv[:, tq * 128:(tq + 1) * 128], hT[:, k, tt16 * 128:(tt16 + 1) * 128], wqkv[:, 2, k, :], k == 0, k == 7, [Twqkv, ThT], tv, k == 7)
                                cp("act", Vtok[:], pv[:], [tv], [TVtok])
                                pw, tw = bank()
                                mm(pw[:], shift_b[:], Vtok[:], True, True, [TVtok, Tc], tw, True)
                                r0 = g4 * 8
                                vv = Vbd[:, r0:r0 + 8, :].rearrange("p (t two) c -> p t two c", two=2)
                                pv3 = pv[:].rearrange("p (t c) -> p t c", t=4)
                                pw3 = pw[:].rearrange("p (t c) -> p t c", t=4)
                                vt3 = Vtok[:].rearrange("p (t c) -> p t c", t=4)
                                cp("pool", vv[0:64, :, 0, 0:64], vt3[0:64, :, 0:64], [TVtok], [TVbd])
                                cp("pool", vv[64:128, :, 1, 64:128], vt3[64:128, :, 64:128], [TVtok], [TVbd])
                                cp("dve", vv[64:128, :, 0, 64:128], pw3[64:128, :, 64:128], [tw], [TVbd])
                                cp("dve", vv[0:64, :, 1, 0:64], pw3[0:64, :, 0:64], [tw], [TVbd])
                            if b == 0 and hp == 0:
                                dump("d_qT", qT[:], [128, SEQ], TqT, BF16)
                                dump("d_Kbd", Kbd[:].rearrange("p a b -> p (a b)"), [128, 32 * 128], TKbd, BF16)
                                dump("d_Vbd", Vbd[:].rearrange("p a b -> p (a b)"), [128, 32 * 128], TVbd, BF16)
                                dump("d_kc", kc[:], [128, NCTX], Tkc, BF16)
                                dump("d_Vcp", Vcp[:].rearrange("p a b c -> p (a b c)"), [128, 512], TVcp, BF16)
                            if stop <= 3:
                                continue
                            steps = []
                            for c in range(4):
                                q0c = c * 512
                                oz = ((banks[3], bankT[3], banks[4], bankT[4]), (banks[5], bankT[5], banks[6], bankT[6]))[c % 2]
                                first = True
                                for hh in range(2):
                                    for cc in range(2):
                                        steps.append(dict(kind="ctx", hh=hh, cc=cc, n=512, q0=q0c, oa=0, oz=oz, first=first, last=False, c=c))
                                        first = False
                                rows = range(c * 8, c * 8 + 8)
                                krows = []
                                for rk in range(32):
                                    val = [r for r in rows if r_start(r) <= rk <= r_start(r) + 7]
                                    if val:
                                        krows.append((rk, val[0], val[-1]))
                                for idx_, (rk, lo, hi) in enumerate(krows):
                                    steps.append(dict(kind="loc", rk=rk, n=(hi - lo + 1) * 64, q0=lo * 64, oa=lo * 64 - q0c, j0=7 + lo - rk, oz=oz,
                                                      first=False, last=idx_ == len(krows) - 1, c=c))
                            sbanks = [(banks[i_], bankT[i_]) for i_ in (0, 1, 2)]

                            def emit_S(i_):
                                st = steps[i_]
                                ps_, tps = sbanks[i_ % 3]
                                n = st["n"]
                                if st["kind"] == "ctx":
                                    hh, cc = st["hh"], st["cc"]
                                    mm(ps_[:], kc[hh * 64:(hh + 1) * 64, cc * 128:(cc + 1) * 128], qT[hh * 64:(hh + 1) * 64, st["q0"]:st["q0"] + 512],
                                       True, True, [Tkc, TqT], tps, True)
                                else:
                                    mm(ps_[:, 0:n], Kbd[:, st["rk"], :], qT[:, st["q0"]:st["q0"] + n], True, False, [TKbd, TqT], tps, False)
                                    mm(ps_[:, 0:n], ident_b[:], TTb[:, hp, st["j0"] * 64:st["j0"] * 64 + n], False, True, [Tc], tps, True)

                            def emit_E(i_):
                                st = steps[i_]
                                ps_, tps = sbanks[i_ % 3]
                                n = st["n"]
                                act(Pt[i_ % 3][:, 0:n], ps_[:, 0:n], AF.Exp, [tps], [TPt[i_ % 3]])

                            def emit_PV(i_):
                                st = steps[i_]
                                n = st["n"]
                                p_, tp = Pt[i_ % 3], TPt[i_ % 3]
                                Oa, TO, Za, TZ = st["oz"]
                                oa = st["oa"]
                                if st["kind"] == "ctx":
                                    mm(Oa[:], Vcp[:, st["cc"], st["hh"], :], p_[:], st["first"], False, [TVcp, tp], TO, False)
                                    mm(Za[:], onespad[:, st["hh"], :], p_[:], st["first"], False, [Tc, tp], TZ, True)
                                else:
                                    mm(Oa[:, oa:oa + n], Vbd[:, st["rk"], :], p_[:, 0:n], False, st["last"], [TVbd, tp], TO, False)
                                    mm(Za[:, oa:oa + n], onesbd[:], p_[:, 0:n], False, st["last"], [Tc, tp], TZ, True)
                                if st["last"]:
                                    q0c_ = st["c"] * 512
                                    P.op("dve", lambda e: e.reciprocal(out=rZ[:], in_=Za[:]), reads=[TZ], writes=[TrZ])
                                    tt_("dve", attT[:, hp, q0c_:q0c_ + 512], Oa[:], rZ[:], ALU.mult, [TO, TZ, TrZ], [TattT])

                            ns_ = len(steps)
                            emit_S(0)
                            emit_S(1)
                            for i_ in range(ns_):
                                emit_E(i_)
                                if i_ + 2 < ns_:
                                    emit_S(i_ + 2)
                                emit_PV(i_)
                        gen["list"] = [0, 1, 2, 3, 4]
                        if b == 0:
                            dump("d_attT", attT[:].rearrange("p a b -> p (a b)"), [128, 4 * SEQ], TattT, BF16)
                        xs1b = xs[1][:].rearrange("p k n -> p (k n)").bitcast(BF16).rearrange("p (a n) -> p a n", a=8)
                        yf = xs[0][:].rearrange("p k n -> p (k n)").rearrange("p (a n) -> p a n", a=4)
                        yb = xs1b[:, 0:4, :]
                        ysq = xs1b[:, 4:8, :]
                        mu = xs[2][:, 0:2, :].rearrange("p a n -> p (a n)")
                        msq = xs[2][:, 2:4, :].rearrange("p a n -> p (a n)")
                        rsc = xs[2][:, 4:6, :].rearrange("p a n -> p (a n)")
                        def fork(t_):
                            n_ = T(t_.name + "_f")
                            n_.w = list(t_.w)
                            n_.r = list(t_.r)
                            return n_
                        Tyf, Tyb, Tysq, Tmu, Tmsq, Trsc = fork(Txs[0]), fork(Txs[1]), fork(Txs[1]), fork(Txs[2]), fork(Txs[2]), fork(Txs[2])
                        for tile in range(4):
                            n0 = tile * 512
                            for j in range(4):
                                pc, tpc = bank()
                                for t in range(31):
                                    mm(pc[:], Dg[:, j, t, :], uT[:, j, n0 + t:n0 + t + 512], t == 0, t == 30, [TDg, TuT], tpc, t == 30)
                                act(yf[:, j, :], pc[:], AF.Identity, [tpc, Tc], [Tyf], bias=cv[:, 0, j:j + 1])
                                act(ysq[:, j, :], pc[:], AF.Square, [tpc, Tc], [Tysq], bias=cv[:, 0, j:j + 1])
                                act(yb[:, j, :], pc[:], AF.Identity, [tpc, Tc], [Tyb], bias=cv[:, 0, j:j + 1])
                            p1, tp1 = bank()
                            p2, tp2 = bank()
                            for j in range(4):
                                mm(p1[:], ones_b[:], yb[:, j, :], j == 0, j == 3, [Tyb, Tc], tp1, j == 3)
                            for j in range(4):
                                mm(p2[:], ones_b[:], ysq[:, j, :], j == 0, j == 3, [Tysq, Tc], tp2, j == 3)
                            ts_("dve", mu[:], p1[:], 1.0 / 512, None, ALU.mult, None, [tp1], [Tmu])
                            tt_("dve", msq[:], mu[:], mu[:], ALU.mult, [Tmu], [Tmsq])
                            stt_("dve", msq[:], p2[:], 1.0 / 512, msq[:], ALU.mult, ALU.subtract, [tp2, Tmsq], [Tmsq])
                            rsqrt_(rsc[:], msq[:], 1.0, [Tmsq], Trsc)
                            for j in range(4):
                                tt_("dve", yf[:, j, :], yf[:, j, :], mu[:], ALU.subtract, [Tyf, Tmu], [Tyf])
                                tt_("dve", yf[:, j, :], yf[:, j, :], rsc[:], ALU.mult, [Tyf, Trsc], [Tyf])
                                act(cTt[:, j, n0:n0 + 512], yf[:, j, :], AF.Silu, [Tyf, Tc], [TcT], bias=cv[:, 2, j:j + 1], scale=cv[:, 1, j:j + 1])
                        if b == 0:
                            dump("d_cT", cTt[:].rearrange("p a b -> p (a b)"), [128, 4 * SEQ], TcT, BF16)
                        P.flush()
                if stop <= 5:
                    break
                if stop < 6 and b > 0:
                    break
                gen["list"] = [0, 1, 2, 3, 4, 5, 6]
                with ExitStack() as s57:
                    wao = sbt(s57, "wao", [128, 4, D], BF16)
                    wco = sbt(s57, "wco", [128, 4, D], BF16)
                    wo = sbt(s57, "wo", [128, 8, D], BF16)
                    Twao, Twco, Two = T("wao"), T("wco"), T("wo")
                    ld("pool", wao[:], w_ao[:, :].rearrange("(k p) n -> p k n", p=128), [Twao])
                    ld("pool", wco[:], w_co[:, :].rearrange("(k p) n -> p k n", p=128), [Twco])
                    ld("pool", wo[:], w_o[:, :].rearrange("(k p) n -> p k n", p=128), [Two])
                    wgs = [sbt(s57, "wgs%d" % i, [128, 2, 8, 128], BF16) for i in range(2)]
                    Twgs = [T("wgs0"), T("wgs1")]
                    sga = sbt(s57, "sga", [128, 512], BF16)
                    sgb = sbt(s57, "sgb", [128, 512], BF16)
                    t1 = sbt(s57, "t1", [128, 512], F32)
                    t2 = sbt(s57, "t2", [128, 512], F32)
                    Tsga, Tsgb, Tt1, Tt2 = T("sga"), T("sgb"), T("t1"), T("t2")
                    mg = sbt(s57, "mg", [128, 8, 512], BF16)
                    Tmg = T("mg")
                    mg2 = sbt(s57, "mg2", [128, 8, 512], BF16)
                    Tmg2 = T("mg2")
                    xc = [sbt(s57, "xc%d" % i, [128, 512], F32) for i in range(3)]
                    Txc = [T("xc%d" % i) for i in range(3)]
                    xm = sbt(s57, "xm", [128, 8, 512], F32)
                    Txm = T("xm")
                    sq2 = [sbt(s57, "sq2%d" % i, [128, 512], BF16) for i in range(2)]
                    Tsq2 = [T("sq20"), T("sq21")]
                    rstd2 = sbt(s57, "rstd2", [128, 512], F32)
                    Trstd2 = T("rstd2")
                    h2f = [sbt(s57, "h2f%d" % i, [128, 512], F32) for i in range(2)]
                    Th2f = [T("h2f0"), T("h2f1")]
                    h2b = sbt(s57, "h2b", [128, 8, 512], BF16)
                    h2l = sbt(s57, "h2l", [128, 8, 512], BF16)
                    Th2l = T("h2l")
                    Th2b = T("h2b")
                    h2tok = [sbt(s57, "h2tok%d" % i, [128, D], BF16) for i in range(2)]
                    Th2tok = [T("h2tok0"), T("h2tok1")]
                    xmtok = [sbt(s57, "xmtok%d" % i, [128, D], F32) for i in range(2)]
                    Txmtok = [T("xmtok0"), T("xmtok1")]
                    L = sbt(s57, "L", [128, 4, 36], F32)
                    gmax = sbt(s57, "gmax", [128, 4], F32)
                    gsh = sbt(s57, "gsh", [128, 4, 4], F32)
                    G1h = sbt(s57, "G1h", [128, 4, 4], F32)
                    ge = sbt(s57, "ge", [128, 4, 4], F32)
                    gsum = sbt(s57, "gsum", [128, 4], F32)
                    pgrp = sbt(s57, "pgrp", [128, 4], F32)
                    mskb = sbt(s57, "mskb", [128, 4, 4], F32)
                    elm = sbt(s57, "elm", [128, 4, 32], F32)
                    elm2 = sbt(s57, "elm2", [128, 4, 32], F32)
                    m1 = sbt(s57, "m1", [128, 4], F32)
                    m2 = sbt(s57, "m2", [128, 4], F32)
                    OH1 = sbt(s57, "OH1", [128, 4, 32], F32)
                    OH2 = sbt(s57, "OH2", [128, 4, 32], F32)
                    Ind = sbt(s57, "Ind", [128, 4, 32], F32)
                    Indb = sbt(s57, "Indb", [128, 4, 32], BF16)
                    Accb = sbt(s57, "Accb", [128, 32], BF16)
                    dd = sbt(s57, "dd", [128, 4], F32)
                    ed = sbt(s57, "ed", [128, 4], F32)
                    w1 = sbt(s57, "w1", [128, 4], F32)
                    w2 = sbt(s57, "w2", [128, 4], F32)
                    tmpR = sbt(s57, "tmpR", [128, 4, 32], F32)
                    prod = sbt(s57, "prod", [128, 4, 32], F32)
                    d1f = sbt(s57, "d1f", [128, 4], F32)
                    d2f = sbt(s57, "d2f", [128, 4], F32)
                    accd = sbt(s57, "accd", [128, 32], F32)
                    TR = T("route")
                    mgs = [mg, mg2]
                    Tmgs = [Tmg, Tmg2]
                    plgs = {}

                    def merge_step(tile, m):
                        n0 = tile * 512
                        tsl = slice(n0, n0 + 512)
                        mg_, Tmg_ = mgs[tile % 2], Tmgs[tile % 2]
                        wg_, twg = wgs[m % 2], Twgs[m % 2]
                        ld("sp", wg_[:].rearrange("p a k n -> p (a k n)"), wgs_d[m], [twg], reads=[Twgd])
                        pya, tya = bank()
                        pyc, tyc = bank()
                        pga, tga = bank()
                        pgb, tgb = bank()
                        for k in range(4):
                            mm(pya[:], wao[:, k, m * 128:(m + 1) * 128], attT[:, k, tsl], k == 0, k == 3, [Twao, TattT], tya, k == 3)
                        for k in range(4):
                            mm(pyc[:], wco[:, k, m * 128:(m + 1) * 128], cTt[:, k, tsl], k == 0, k == 3, [Twco, TcT], tyc, k == 3)
                        for k in range(8):
                            mm(pga[:], wg_[:, 0, k, :], hT[:, k, tsl], k == 0, k == 7, [twg, ThT], tga, k == 7)
                        for k in range(8):
                            mm(pgb[:], wg_[:, 1, k, :], hT[:, k, tsl], k == 0, k == 7, [twg, ThT], tgb, k == 7)
                        act(sga[:], pga[:], AF.Sigmoid, [tga], [Tsga])
                        act(sgb[:], pgb[:], AF.Sigmoid, [tgb], [Tsgb])
                        tt_("dve", t1[:], pya[:], sga[:], ALU.mult, [tya, Tsga], [Tt1])
                        tt_("dve", t2[:], pyc[:], sgb[:], ALU.mult, [tyc, Tsgb], [Tt2])
                        tt_("dve", mg_[:, m, :], t1[:], t2[:], ALU.add, [Tt1, Tt2], [Tmg_])

                    def wo_step(tile, m):
                        n0 = tile * 512
                        mg_, Tmg_ = mgs[tile % 2], Tmgs[tile % 2]
                        xc_, txc = xc[m % 3], Txc[m % 3]
                        ld("sp", xc_[:], xT[b, m, :, n0:n0 + 512], [txc])
                        po, tpo = bank()
                        for k in range(8):
                            mm(po[:], wo[:, k, m * 128:(m + 1) * 128], mg_[:, k, :], k == 0, k == 7, [Two, Tmg_], tpo, k == 7)
                        stt_("dve", xm[:, m, :], po[:], mod[:, 16 + m, b:b + 1], xc_[:], ALU.mult, ALU.add, [tpo, Tmod, txc], [Txm])

                    def norm2_step(tile):
                        pss, tpss = bank()
                        for m in range(8):
                            act(sq2[m % 2][:], xm[:, m, :], AF.Square, [Txm], [Tsq2[m % 2]])
                            mm(pss[:], ones_b[:], sq2[m % 2][:], m == 0, m == 7, [Tsq2[m % 2], Tc], tpss, True)
                        rsqrt_(rstd2[:], pss[:], 1.0 / D, [tpss], Trstd2)


                    def h2_step(tile, k):
                        hf, thf = h2f[k % 2], Th2f[k % 2]
                        tt_("dve", hf[:], xm[:, k, :], rstd2[:], ALU.mult, [Txm, Trstd2], [thf])
                        act(hf[:], hf[:], AF.Identity, [thf, Tmod], [thf], bias=mod[:, 24 + k, b:b + 1], scale=A2[:, k, b:b + 1])
                        cp("act", h2b[:, k, :], hf[:], [thf], [Th2b])
                        tt_("dve", h2l[:, k, :], hf[:], h2b[:, k, :], ALU.subtract, [thf, Th2b], [Th2l])

                    def router_step(tile):
                        pl_, tpl_ = bank()
                        plgs[tile] = (pl_, tpl_)
                        for sub in range(4):
                            ssl = slice(sub * 128, (sub + 1) * 128)
                            o_ = pl_[:, sub * 36:(sub + 1) * 36]
                            for k in range(8):
                                mm(o_, h2b[:, k, ssl], wrh[:, k, :], k == 0, False, [Th2b, Tc], tpl_, False)
                                mm(o_, h2b[:, k, ssl], wrl[:, k, :], False, False, [Th2b, Tc], tpl_, False)
                                mm(o_, h2l[:, k, ssl], wrh[:, k, :], False, k == 7, [Th2l, Tc], tpl_, k == 7)

                    def routing_step(tile):
                        gt = b * 4 + tile
                        pl_, tpl_ = plgs[tile]
                        tt_("dve", L[:], pl_[:, 0:144].rearrange("p (s e) -> p s e", s=4), br[:], ALU.add, [tpl_, Tc], [TR])
                        gl = L[:, :, 0:4]
                        red("dve", gmax[:], gl, ALU.max, [TR], [TR])
                        tt_("dve", gsh[:], gl, gmax[:].unsqueeze(2).to_broadcast([128, 4, 4]), ALU.subtract, [TR], [TR])
                        ts_("dve", G1h[:], gsh[:], 0.0, None, ALU.is_equal, None, [TR], [TR])
                        act(ge[:], gsh[:], AF.Exp, [TR], [TR])
                        red("dve", gsum[:], ge[:], ALU.add, [TR], [TR])
                        P.op("dve", lambda e: e.reciprocal(out=pgrp[:], in_=gsum[:]), reads=[TR], writes=[TR])
                        ts_("dve", mskb[:], G1h[:], BIG, -BIG, ALU.mult, ALU.add, [TR], [TR])
                        el4 = L[:, :, 4:36].rearrange("p s (g j) -> p s g j", g=4)
                        elm4 = elm[:].rearrange("p s (g j) -> p s g j", g=4)
                        tt_("dve", elm4, el4, G1h[:].unsqueeze(3).to_broadcast([128, 4, 4, 8]), ALU.mult, [TR], [TR])
                        tt_("dve", elm4, elm4, mskb[:].unsqueeze(3).to_broadcast([128, 4, 4, 8]), ALU.add, [TR], [TR])
                        red("dve", m1[:], elm[:], ALU.max, [TR], [TR])
                        tt_("dve", OH1[:], elm[:], m1[:].unsqueeze(2).to_broadcast([128, 4, 32]), ALU.is_equal, [TR], [TR])
                        stt_("dve", elm2[:], OH1[:], -BIG, elm[:], ALU.mult, ALU.add, [TR], [TR])
                        red("dve", m2[:], elm2[:], ALU.max, [TR], [TR])
                        tt_("dve", OH2[:], elm2[:], m2[:].unsqueeze(2).to_broadcast([128, 4, 32]), ALU.is_equal, [TR], [TR])
                        tt_("dve", dd[:], m2[:], m1[:], ALU.subtract, [TR], [TR])
                        act(ed[:], dd[:], AF.Exp, [TR], [TR])
                        ts_("dve", w1[:], ed[:], 1.0, None, ALU.add, None, [TR], [TR])
                        P.op("dve", lambda e: e.reciprocal(out=w1[:], in_=w1[:]), reads=[TR], writes=[TR])
                        tt_("dve", w2[:], ed[:], w1[:], ALU.mult, [TR], [TR])
                        tt_("dve", Wk[:, gt * 4:(gt + 1) * 4, 0], w1[:], pgrp[:], ALU.mult, [TR], [TWk])
                        tt_("dve", Wk[:, gt * 4:(gt + 1) * 4, 1], w2[:], pgrp[:], ALU.mult, [TR], [TWk])
                        tt_("dve", Ind[:], OH1[:], OH2[:], ALU.add, [TR], [TR])
                        cp("dve", Indb[:], Ind[:], [TR], [TR])
                        cp("dve", Accb[:], Acc[:], [TAcc], [TR])
                        pr, tpr = bank()
                        for sub in range(4):
                            ops_ = [(ones_b, Accb[:])] + [(ones_b, Indb[:, s_, :]) for s_ in range(sub)] + [(utri_b, Indb[:, sub, :])]
                            for i_, (l_, r_) in enumerate(ops_):
                                mm(pr[:, sub * 32:(sub + 1) * 32], l_[:], r_, i_ == 0, i_ == len(ops_) - 1, [TR, TAcc, Tc], tpr, i_ == len(ops_) - 1)
                        tt_("dve", tmpR[:], pr[:, 0:128].rearrange("p (s e) -> p s e", s=4), ecap[:], ALU.add, [tpr, Tc], [TR])
                        tt_("dve", prod[:], tmpR[:], OH1[:], ALU.mult, [TR], [TR])
                        red("dve", d1f[:], prod[:], ALU.add, [TR], [TR])
                        tt_("dve", prod[:], tmpR[:], OH2[:], ALU.mult, [TR], [TR])
                        red("dve", d2f[:], prod[:], ALU.add, [TR], [TR])
                        cp("dve", Dk[:, gt * 4:(gt + 1) * 4, 0], d1f[:], [TR], [TDk])
                        cp("dve", Dk[:, gt * 4:(gt + 1) * 4, 1], d2f[:], [TR], [TDk])
                        red("dve", accd[:], Ind[:].rearrange("p s e -> p e s"), ALU.add, [TR], [TR])
                        tt_("dve", Acc[:], Acc[:], accd[:], ALU.add, [TR, TAcc], [TAcc])

                    def trans_step(tile, sub):
                        n0 = tile * 512
                        gt = b * 4 + tile
                        ssl = slice(sub * 128, (sub + 1) * 128)
                        for k in range(8):
                            tr(pbf[:, k * 128:(k + 1) * 128], h2b[:, k, ssl], ident_b[:], [Th2b, Tc], Tpbf, k == 7)
                        ht, tht = h2tok[sub % 2], Th2tok[sub % 2]
                        cp("act", ht[:], pbf[:], [Tpbf], [tht])
                        for kk in range(2):
                            off = Dk[:, gt * 4 + sub, kk:kk + 1]
                            P.dma("pool", (lambda o_, h_: (lambda e: e.indirect_dma_start(
                                out=Xs[:, :], out_offset=bass.IndirectOffsetOnAxis(ap=o_, axis=0), in_=h_, in_offset=None)))(off, ht[:]),
                                reads=[tht, TDk], writes=[TXs])
                        pxa, tpxa = bank()
                        pxb, tpxb = bank()
                        for k in range(4):
                            tr(pxa[:, k * 128:(k + 1) * 128], xm[:, k, ssl], ident_f[:], [Txm, Tc], tpxa, k == 3)
                        for k in range(4):
                            tr(pxb[:, k * 128:(k + 1) * 128], xm[:, 4 + k, ssl], ident_f[:], [Txm, Tc], tpxb, k == 3)
                        xt_, txt = xmtok[sub % 2], Txmtok[sub % 2]
                        cp("dve", xt_[:, 0:512], pxa[:], [tpxa], [txt])
                        cp("act", xt_[:, 512:1024], pxb[:], [tpxb], [txt])
                        tok0 = b * SEQ + n0 + sub * 128
                        ld("sp", xmid[tok0:tok0 + 128, :], xt_[:], [Txmid], reads=[txt])


                    def post_steps(tile):
                        st = [(lambda m=m: wo_step(tile, m)) for m in range(8)]
                        st.append(lambda: norm2_step(tile))
                        st += [(lambda k=k: h2_step(tile, k)) for k in range(8)]
                        st.append(lambda: router_step(tile))
                        st.append(lambda: routing_step(tile))
                        st += [(lambda sub=sub: trans_step(tile, sub)) for sub in range(4)]
                        return st

                    for m in range(8):
                        merge_step(0, m)
                    for tile in range(4):
                        ps_ = post_steps(tile)
                        ms_ = [(lambda m=m: merge_step(tile + 1, m)) for m in range(8)] if tile < 3 else []
                        pi_, mi_ = 0, 0
                        while pi_ < len(ps_) or mi_ < len(ms_):
                            for _ in range(3):
                                if pi_ < len(ps_):
                                    ps_[pi_]()
                                    pi_ += 1
                            if mi_ < len(ms_):
                                ms_[mi_]()
                                mi_ += 1
                    P.flush()
                gen["list"] = [0, 1, 2, 3, 4]
                if stop <= 6:
                    break
        if dbg and stop > 5.4:
            dump("d_Dk", Dk[:].rearrange("p a b -> p (a b)"), [128, 128], TDk, I32)
            dump("d_Wk", Wk[:].rearrange("p a b -> p (a b)"), [128, 128], TWk)
            d_ = dout("d_xmid", [SEQ, D])
            ld("sp", d_, xmid[0:SEQ, :], [T()], reads=[Txmid])
        if stop <= 6:
            finish()
            return nc, dbg_out
        P.flush()
        ps_es.close()
        ps2 = ExitStack()
        banks2 = [ps2.enter_context(nc.psum_tensor("qb%d" % i, [128, 512], F32)) for i in range(6)]
        bankT2 = [T("qb%d" % i, excl=True) for i in range(6)]
        pbf2 = [ps2.enter_context(nc.psum_tensor("qbf%d" % i, [128, 1024], BF16)) for i in range(2)]
        Tpbf2 = [T("qbf0", excl=True), T("qbf1", excl=True)]
        gen["banks"], gen["bankT"], gen["list"], gen["i"] = banks2, bankT2, [0, 1, 2, 3, 4, 5], 0
        with ExitStack() as s2:
            wge = [sbt(s2, "wge%d" % i, [128, 8, 512], BF16) for i in range(2)]
            wue = [sbt(s2, "wue%d" % i, [128, 8, 512], BF16) for i in range(2)]
            wde = [sbt(s2, "wde%d" % i, [128, 4, D], BF16) for i in range(2)]
            Twe = [T("we0"), T("we1")]
            xg = [sbt(s2, "xg%d" % i, [128, 4, D], BF16) for i in range(3)]
            Txg = [T("xg%d" % i) for i in range(3)]
            XT = [sbt(s2, "XT%d" % i, [128, 8, 512], BF16) for i in range(2)]
            TXT = [T("XT0"), T("XT1")]
            sgf = [sbt(s2, "sgf%d" % i, [128, 512], F32) for i in range(2)]
            Tsgf = [T("sgf0"), T("sgf1")]
            AT = [sbt(s2, "AT%d" % i, [128, 4, 512], BF16) for i in range(2)]
            TAT = [T("AT0"), T("AT1")]
            yo = [sbt(s2, "yo%d" % i, [128, D], F32) for i in range(4)]
            Tyo = [T("yo%d" % i) for i in range(4)]
            ngrp = cap // 512
            groups = [(e_, g_) for e_ in range(NE) for g_ in range(ngrp)]
            cnt2 = {"tp": 0, "yi": 0}

            def ld_w(e_):
                we_ = e_ % 2
                ld("pool", wge[we_][:], w_g[e_].rearrange("(k p) n -> p k n", p=128), [Twe[we_]])
                ld("pool", wue[we_][:], w_u[e_].rearrange("(k p) n -> p k n", p=128), [Twe[we_]])
                ld("pool", wde[we_][:], w_d[e_].rearrange("(k p) n -> p k n", p=128), [Twe[we_]])

            def ld_x(gi):
                e_, g_ = groups[gi]
                s0 = e_ * cap + g_ * 512
                ld("sp", xg[gi % 3][:], Xs[s0:s0 + 512, :].rearrange("(blk p) d -> p blk d", p=128), [Txg[gi % 3]], reads=[TXs])

            def st_T(gi):
                xg_, txg = xg[gi % 3], Txg[gi % 3]
                xt_, txt = XT[gi % 2], TXT[gi % 2]
                for k2 in range(4):
                    pb_, tpb_ = pbf2[cnt2["tp"] % 2], Tpbf2[cnt2["tp"] % 2]
                    cnt2["tp"] += 1
                    for kk in range(2):
                        k = k2 * 2 + kk
                        for blk in range(4):
                            tr(pb_[:, kk * 512 + blk * 128:kk * 512 + (blk + 1) * 128], xg_[:, blk, k * 128:(k + 1) * 128], ident_b[:],
                               [txg, Tc], tpb_, kk == 1 and blk == 3)
                    cp("act" if k2 % 2 == 0 else "dve", xt_[:, k2 * 2:k2 * 2 + 2, :].rearrange("p a b -> p (a b)"), pb_[:], [tpb_], [txt])

            def st_GU(gi):
                e_, g_ = groups[gi]
                we_ = e_ % 2
                xt_, txt = XT[gi % 2], TXT[gi % 2]
                at_, tat = AT[gi % 2], TAT[gi % 2]
                for j in range(4):
                    pg, tpg = bank()
                    pu, tpu = bank()
                    for k in range(8):
                        mm(pg[:], wge[we_][:, k, j * 128:(j + 1) * 128], xt_[:, k, :], k == 0, k == 7, [Twe[we_], txt], tpg, k == 7)
                    for k in range(8):
                        mm(pu[:], wue[we_][:, k, j * 128:(j + 1) * 128], xt_[:, k, :], k == 0, k == 7, [Twe[we_], txt], tpu, k == 7)
                    sg_, tsg_ = sgf[j % 2], Tsgf[j % 2]
                    act(sg_[:], pg[:], AF.Silu, [tpg], [tsg_])
                    tt_("dve", at_[:, j, :], pu[:], sg_[:], ALU.mult, [tpu, tsg_], [tat])

            def st_D(gi):
                e_, g_ = groups[gi]
                we_ = e_ % 2
                s0 = e_ * cap + g_ * 512
                at_, tat = AT[gi % 2], TAT[gi % 2]
                for blk in range(4):
                    yo_, tyo = yo[cnt2["yi"] % 4], Tyo[cnt2["yi"] % 4]
                    cnt2["yi"] += 1
                    for half in range(2):
                        py, tpy = bank()
                        for j in range(4):
                            mm(py[:], at_[:, j, blk * 128:(blk + 1) * 128], wde[we_][:, j, half * 512:(half + 1) * 512], j == 0, j == 3,
                               [tat, Twe[we_]], tpy, j == 3)
                        cp("act" if half == 0 else "dve", yo_[:, half * 512:(half + 1) * 512], py[:], [tpy], [tyo])
                    ld("sp", Ys[s0 + blk * 128:s0 + (blk + 1) * 128, :], yo_[:], [TYs], reads=[tyo])

            cnt_i = sbt(s2, "cnt_i", [128, NE], I32)
            Accb2 = sbt(s2, "Accb2", [128, NE], BF16)
            Tcnt = T("cnt")
            cp("dve", Accb2[:], Acc[:], [TAcc], [Tcnt])
            pbc, tbc = bank()
            mm(pbc[:, 0:NE], ones_b[:], Accb2[:], True, True, [Tcnt, Tc], tbc, True)
            cp("dve", cnt_i[:], pbc[:, 0:NE], [tbc], [Tcnt])
            P.cnt_ap = cnt_d
            ev_ = P.dma("sp", lambda e: e.dma_start(out=cnt_d[0:1, :], in_=cnt_i[0:1, :]), reads=[Tcnt], writes=[T()])
            P.cnt_event = (ev_[0], ev_[1])

            def cond_of(gi, what="x"):
                e_, g_ = groups[gi]
                return None if g_ == 0 else (e_, g_ * 512)

            ng = len(groups)
            ld_w(0)
            ld_x(0)
            P.set_cond(cond_of(1, "x"))
            ld_x(1)
            P.set_cond(None)
            st_T(0)
            for gi in range(ng):
                e_, g_ = groups[gi]
                if g_ == 0 and e_ + 1 < NE:
                    P.set_cond(None)
                    ld_w(e_ + 1)
                if gi + 2 < ng:
                    P.set_cond(cond_of(gi + 2, "x"))
                    ld_x(gi + 2)
                P.set_cond(cond_of(gi, "g"))
                st_GU(gi)
                if gi + 1 < ng:
                    P.set_cond(cond_of(gi + 1, "t"))
                    st_T(gi + 1)
                P.set_cond(cond_of(gi, "d"))
                st_D(gi)
            P.set_cond(None)
            P.flush()
        if stop <= 7:
            finish()
            return nc, dbg_out
        with ExitStack() as s3:
            fn = sbt(s3, "fn", [128, D], F32)
            G2 = sbt(s3, "G2", [128, D], F32)
            dgh = sbt(s3, "dgh", [128, 128], BF16)
            dgl = sbt(s3, "dgl", [128, 128], BF16)
            dgf = sbt(s3, "dgf", [128, 128], F32)
            dgr = sbt(s3, "dgr", [128, 128], F32)
            Tfn, TG2, Tdg = T("fn"), T("G2"), T("dg")
            NB3 = 4
            Y1 = [sbt(s3, "Y1%d" % i, [128, D], F32) for i in range(NB3)]
            Y2 = [sbt(s3, "Y2%d" % i, [128, D], F32) for i in range(NB3)]
            X3 = [sbt(s3, "X3%d" % i, [128, D], F32) for i in range(NB3)]
            F3 = [sbt(s3, "F3%d" % i, [128, D], F32) for i in range(NB3)]
            J3 = sbt(s3, "J3", [128, D], F32)
            ssq = [sbt(s3, "ssq%d" % i, [128, 1], F32) for i in range(NB3)]
            TY1, TY2, TX3, TF3, Tss = [[T() for _ in range(NB3)] for _ in range(5)]
            TJ3 = T("J3")
            ld("sp", fn[:], fnorm[:, :], [Tfn])
            tiles3 = [(b, tl) for b in range(NB) for tl in range(16)]

            def p1(it):
                b, tl = tiles3[it]
                i2 = it % NB3
                if tl == 0:
                    for k in range(8):
                        ts_("dve", dgf[:], ident_f[:], mod[:, 40 + k, b:b + 1], None, ALU.mult, None, [Tc, Tmod, Tdg], [Tdg])
                        cp("dve", dgh[:], dgf[:], [Tdg], [Tdg])
                        tt_("dve", dgr[:], dgf[:], dgh[:], ALU.subtract, [Tdg], [Tdg])
                        cp("dve", dgl[:], dgr[:], [Tdg], [Tdg])
                        pb, tb = bank()
                        mm(pb[:, 0:128], ones_b[:], dgh[:], True, False, [Tc, Tdg], tb, False)
                        mm(pb[:, 0:128], ones_b[:], dgl[:], False, True, [Tc, Tdg], tb, True)
                        cp("act", G2[:, k * 128:(k + 1) * 128], pb[:, 0:128], [tb] + TF3, [TG2])

                idx = (b * 4 + tl // 4) * 4 + tl % 4
                tok0 = b * SEQ + tl * 128
                for kk, (Y_, TY_) in enumerate(((Y1[i2], TY1[i2]), (Y2[i2], TY2[i2]))):
                    off = Dk[:, idx, kk:kk + 1]
                    P.dma("pool", (lambda o_, y_: (lambda e: e.indirect_dma_start(
                        out=y_, out_offset=None, in_=Ys[:, :], in_offset=bass.IndirectOffsetOnAxis(ap=o_, axis=0))))(off, Y_[:]),
                        reads=[TYs, TDk], writes=[TY_])
                ld("sp", X3[i2][:], xmid[tok0:tok0 + 128, :], [TX3[i2]], reads=[Txmid])
                act(F3[i2][:], Y1[i2][:], AF.Copy, [TY1[i2], TWk], [TF3[i2]], scale=Wk[:, idx, 0:1])
                stt_("dve", F3[i2][:], Y2[i2][:], Wk[:, idx, 1:2], F3[i2][:], ALU.mult, ALU.add, [TY2[i2], TWk, TF3[i2]], [TF3[i2]])
                tt_("dve", F3[i2][:], F3[i2][:], G2[:], ALU.mult, [TF3[i2], TG2], [TF3[i2]])
                tt_("dve", X3[i2][:], X3[i2][:], F3[i2][:], ALU.add, [TX3[i2], TF3[i2]], [TX3[i2]])
                mset("dve", ssq[i2][:], 0.0, [Tss[i2]])
                act(J3[:], X3[i2][:], AF.Square, [TX3[i2], Tss[i2]], [TJ3, Tss[i2]], accum=ssq[i2][:])
                act(ssq[i2][:], ssq[i2][:], AF.Sqrt, [Tss[i2], Tc], [Tss[i2]], bias=epsc[:, 0:1], scale=1.0 / D)

            def p2(it):
                b, tl = tiles3[it]
                i2 = it % NB3
                tok0 = b * SEQ + tl * 128
                P.op("dve", lambda e: e.reciprocal(out=ssq[i2][:], in_=ssq[i2][:]), reads=[Tss[i2]], writes=[Tss[i2]])
                stt_("dve", X3[i2][:], X3[i2][:], ssq[i2][:, 0:1], fn[:], ALU.mult, ALU.mult, [TX3[i2], Tss[i2], Tfn], [TX3[i2]])
                ld("sp", out[tok0:tok0 + 128, :], X3[i2][:], [Tout], reads=[TX3[i2]])

            n3 = len(tiles3)
            p1(0)
            for it in range(n3):
                if it + 1 < n3:
                    p1(it + 1)
                p2(it)
            P.flush()
        finish()
        return nc, dbg_out


def host_prep(inputs, core):
    f = np.float32
    bs = slice(NB * core, NB * core + NB)
    x = np.asarray(inputs["x"][bs], f)
    ctx = np.asarray(inputs["ctx"][bs], f)
    m = {}
    m["xT"] = np.ascontiguousarray(x.transpose(0, 2, 1)).reshape(NB, 8, 128, SEQ)
    m["ctxT"] = np.ascontiguousarray(ctx.transpose(0, 2, 1)).reshape(NB, 8, 128, NCTX)
    cc = np.concatenate([np.asarray(inputs["c"][bs], f), np.asarray(inputs["c_ctx"], f)[None]], 0)
    m["cT"] = np.ascontiguousarray(cc.reshape(5, 8, 128).transpose(2, 1, 0)).reshape(128, 40)
    return m


def host_shared(inputs):
    f = np.float32
    g = lambda k: np.asarray(inputs[k], f)[0]
    s = {}
    s["w_mod"] = np.ascontiguousarray(g("w_mod"))
    s["b_modT"] = np.ascontiguousarray(g("b_mod").reshape(48, 128).T)
    s["nmixT"] = np.ascontiguousarray(g("norm_mix").reshape(8, 128).T)
    s["nffnT"] = np.ascontiguousarray(g("norm_ffn").reshape(8, 128).T)
    s["w_in"] = np.ascontiguousarray(g("w_in"))
    rpb = g("rpb")
    p = np.arange(128)
    half, ck = p // 64, p % 64
    cq = np.arange(64)
    c_start = np.clip(cq - 8, 0, 48)
    mask = (ck[:, None] >= c_start[None, :]) & (ck[:, None] < c_start[None, :] + 16)
    dc = np.clip(ck[:, None] - cq[None, :] + 15, 0, 30)
    tt = np.empty((128, 4, 15, 64), f)
    for hp in range(4):
        h = 2 * hp + half
        for jj in range(15):
            v = rpb[h[:, None], 14 - jj, dc]
            tt[:, hp, jj, :] = np.where(mask, v, f(NEG))
    s["tt"] = tt.reshape(128, -1)
    s["w_att_out"] = np.ascontiguousarray(g("w_att_out"))
    s["w_conv_out"] = np.ascontiguousarray(g("w_conv_out"))
    s["w_o"] = np.ascontiguousarray(g("w_o"))
    s["cwT"] = np.ascontiguousarray(g("conv_w").reshape(31, 4, 128).transpose(2, 1, 0)).reshape(128, -1)
    cvec = np.stack([g("conv_b"), g("conv_ln_g"), g("conv_ln_b")], 0)
    s["cvec"] = np.ascontiguousarray(cvec.reshape(3, 4, 128).transpose(2, 0, 1)).reshape(128, 12)
    wrc = np.concatenate([g("w_router_group"), g("w_router_expert")], 1)
    s["wr"] = np.ascontiguousarray(wrc.reshape(8, 128, 36).transpose(1, 0, 2)).reshape(128, -1)
    brc = np.concatenate([g("b_router_group"), g("b_router_expert")], 0)
    s["br"] = np.ascontiguousarray(np.broadcast_to(brc[None, None, :], (128, 4, 36))).reshape(128, -1)
    s["ecap"] = np.ascontiguousarray(np.broadcast_to((np.arange(32, dtype=f) * CAP)[None, None, :], (128, 4, 32))).reshape(128, -1)
    s["w_exp_gate"] = np.ascontiguousarray(g("w_exp_gate"))
    s["w_exp_up"] = np.ascontiguousarray(g("w_exp_up"))
    s["w_exp_down"] = np.ascontiguousarray(g("w_exp_down"))
    s["fnorm"] = np.ascontiguousarray(np.broadcast_to(np.asarray(inputs["final_norm"], f)[None, :], (128, D)))
    return s


def kernel(**inputs):
    nc, _ = build()
    shared = host_shared(inputs)
    in_maps = []
    for core in range(8):
        m = dict(shared)
        m.update(host_prep(inputs, core))
        in_maps.append(m)
    res = run_bass_kernel_spmd(nc, in_maps, core_ids=list(range(8)))
    outs = [np.asarray(r["out"]).reshape(NB, SEQ, D) for r in res.results]
    return np.concatenate(outs, 0).astype(np.float32)
```
